# Optimizing a Trainium2 kernel written in Bass

```python
import jax, jax.numpy as jnp
from jax import lax
import numpy as np

D_MODEL = 1024
BATCH = 16
SEQ = 4096
DEPTH = 1

N_HEADS_ATTN = 8
HEAD_DIM = 64
ATTN_WIDTH = N_HEADS_ATTN * HEAD_DIM
IDX_HEADS = 8
IDX_DIM = 32
TOPK_MAX = 256
Q_BLOCK = 128
CONV_WIDTH = D_MODEL - ATTN_WIDTH
CONV_GROUPS = 8
CONV_K = 3
MIX_WIDTH = ATTN_WIDTH + CONV_WIDTH
ROPE_THETA = 500000.0
ROPE_DIM = HEAD_DIM // 4
D_FF = ((-(-8 * D_MODEL // 3)) + 255) // 256 * 256
DEEPNORM_ALPHA = (2.0 * DEPTH) ** 0.25
DEEPNORM_BETA = (8.0 * DEPTH) ** -0.25
EPS = 1e-5
NEG_INF = -1e30

IN_SPLITS = (ATTN_WIDTH, HEAD_DIM, HEAD_DIM, IDX_HEADS * IDX_DIM, IDX_DIM, IDX_HEADS,
             CONV_WIDTH, CONV_WIDTH, CONV_WIDTH)
IN_COLS = sum(IN_SPLITS)

kernel_name = "hymba_dsa_shortconv_deepnorm_adaln"


def rope_tables(seq_len):
    pos = jnp.arange(seq_len, dtype=jnp.float32)
    inv = ROPE_THETA ** (-(jnp.arange(0, ROPE_DIM, 2, dtype=jnp.float32) / ROPE_DIM))
    ang = pos[:, None] * inv[None, :]
    return jnp.cos(ang)[:, None, :], jnp.sin(ang)[:, None, :]


def partial_rope(x, cos, sin):
    cos = cos.astype(x.dtype)
    sin = sin.astype(x.dtype)
    half = ROPE_DIM // 2
    x1, x2, rest = x[..., :half], x[..., half:ROPE_DIM], x[..., ROPE_DIM:]
    return jnp.concatenate([x1 * cos - x2 * sin, x2 * cos + x1 * sin, rest], axis=-1)


def layer_norm(x, g, b):
    xf = x.astype(jnp.float32)
    mu = jnp.mean(xf, axis=-1, keepdims=True)
    var = jnp.mean(jnp.square(xf - mu), axis=-1, keepdims=True)
    y = (xf - mu) * lax.rsqrt(var + EPS) * g.astype(jnp.float32) + b.astype(jnp.float32)
    return y.astype(x.dtype)


def group_rms_norm(x, g, n_groups):
    bsz, s, w = x.shape
    xf = x.astype(jnp.float32).reshape(bsz, s, n_groups, w // n_groups)
    xf = xf * lax.rsqrt(jnp.mean(jnp.square(xf), axis=-1, keepdims=True) + EPS)
    return (xf.reshape(bsz, s, w) * g.astype(jnp.float32)).astype(x.dtype)


def modulate(x, shift, scale):
    return x * (1.0 + scale[:, None, :]) + shift[:, None, :]


def sparse_indexer_attention(q, k, v, qi, ki, wi, k_sel):
    bsz, s, h, dh = q.shape
    n_blk = s // Q_BLOCK
    attn_scale = HEAD_DIM ** -0.5
    key_pos = jnp.arange(s)
    ki32 = ki.astype(jnp.float32)

    def to_blocks(a):
        return jnp.moveaxis(a.reshape((bsz, n_blk, Q_BLOCK) + a.shape[2:]), 1, 0)

    def block_fn(args):
        q_b, qi_b, w_b, t_b = args
        rel = jax.nn.relu(jnp.einsum('bqhd,bsd->bqhs', qi_b.astype(jnp.float32), ki32))
        score = jnp.einsum('bqhs,bqh->bqs', rel, w_b.astype(jnp.float32))
        causal = key_pos[None, :] <= t_b[:, None]
        score = jnp.where(causal[None], score, NEG_INF)
        _, sel = lax.top_k(score, k_sel)
        valid = sel <= t_b[None, :, None]
        kg = jax.vmap(lambda kb, ib: kb[ib])(k, sel)
        vg = jax.vmap(lambda vb, ib: vb[ib])(v, sel)
        logits = jnp.einsum('bqhd,bqkd->bhqk', q_b, kg).astype(jnp.float32) * attn_scale
        logits = jnp.where(valid[:, None], logits, NEG_INF)
        p = jax.nn.softmax(logits, axis=-1).astype(v.dtype)
        return jnp.einsum('bhqk,bqkd->bqhd', p, vg)

    t_blocks = key_pos.reshape(n_blk, Q_BLOCK)
    out = lax.map(block_fn, (to_blocks(q), to_blocks(qi), to_blocks(wi), t_blocks))
    return jnp.moveaxis(out, 0, 1).reshape(bsz, s, h, dh)


def token_mixer(h, w_in, conv_w, attn_norm_g, conv_norm_g, w_out, cos, sin):
    bsz, s, _ = h.shape
    k_sel = min(TOPK_MAX, s // 4)
    proj = h @ w_in
    offsets = np.cumsum(IN_SPLITS)[:-1].tolist()
    q, k, v, qi, ki, wi, gate_b, gate_c, hc = jnp.split(proj, offsets, axis=-1)

    q = partial_rope(q.reshape(bsz, s, N_HEADS_ATTN, HEAD_DIM), cos, sin)
    k = partial_rope(k[:, :, None, :], cos, sin)[:, :, 0, :]
    qi = partial_rope(qi.reshape(bsz, s, IDX_HEADS, IDX_DIM), cos, sin)
    ki = partial_rope(ki[:, :, None, :], cos, sin)[:, :, 0, :]
    wi = wi * (IDX_HEADS ** -0.5 * IDX_DIM ** -0.5)
    attn = sparse_indexer_attention(q, k, v, qi, ki, wi, k_sel).reshape(bsz, s, ATTN_WIDTH)
    attn = group_rms_norm(attn, attn_norm_g, N_HEADS_ATTN)

    u = gate_c * hc
    u_pad = jnp.pad(u, ((0, 0), (CONV_K - 1, 0), (0, 0)))
    conv = sum(conv_w[j] * u_pad[:, j:j + s, :] for j in range(CONV_K))
    conv_out = group_rms_norm(gate_b * conv, conv_norm_g, CONV_GROUPS)

    mixed = jnp.concatenate([attn, conv_out], axis=-1)
    return mixed @ w_out


def swiglu(h, w_gate, w_up, w_down):
    return (jax.nn.silu(h @ w_gate) * (h @ w_up)) @ w_down


def setup_inputs(seed: int = 0) -> dict:
    key = jax.random.key(seed)
    ks = jax.random.split(key, 17)
    n = jax.random.normal
    f32 = jnp.float32
    L = DEPTH
    return {
        "x": n(ks[0], (BATCH, SEQ, D_MODEL), f32),
        "c": n(ks[1], (BATCH, D_MODEL), f32),
        "w_ada": n(ks[2], (L, D_MODEL, 6 * D_MODEL), f32) * (D_MODEL ** -0.5),
        "b_ada": n(ks[3], (L, 6 * D_MODEL), f32) * 0.02,
        "w_in": n(ks[4], (L, D_MODEL, IN_COLS), f32) * (D_MODEL ** -0.5),
        "conv_w": n(ks[5], (L, CONV_K, CONV_WIDTH), f32) * (CONV_K ** -0.5),
        "attn_norm_g": 1.0 + 0.1 * n(ks[6], (L, ATTN_WIDTH), f32),
        "conv_norm_g": 1.0 + 0.1 * n(ks[7], (L, CONV_WIDTH), f32),
        "w_out": n(ks[8], (L, MIX_WIDTH, D_MODEL), f32) * (MIX_WIDTH ** -0.5) * DEEPNORM_BETA,
        "ln1_g": 1.0 + 0.1 * n(ks[9], (L, D_MODEL), f32),
        "ln1_b": 0.02 * n(ks[10], (L, D_MODEL), f32),
        "w_gate": n(ks[11], (L, D_MODEL, D_FF), f32) * (D_MODEL ** -0.5),
        "w_up": n(ks[12], (L, D_MODEL, D_FF), f32) * (D_MODEL ** -0.5),
        "w_down": n(ks[13], (L, D_FF, D_MODEL), f32) * (D_FF ** -0.5) * DEEPNORM_BETA,
        "ln2_g": 1.0 + 0.1 * n(ks[14], (L, D_MODEL), f32),
        "ln2_b": 0.02 * n(ks[15], (L, D_MODEL), f32),
    }


def reference(x, c, w_ada, b_ada, w_in, conv_w, attn_norm_g, conv_norm_g, w_out,
              ln1_g, ln1_b, w_gate, w_up, w_down, ln2_g, ln2_b):
    s = x.shape[1]
    cos, sin = rope_tables(s)
    c_act = jax.nn.silu(c)
    for l in range(DEPTH):
        mod = c_act @ w_ada[l] + b_ada[l]
        sh1, sc1, g1, sh2, sc2, g2 = jnp.split(mod, 6, axis=-1)
        mix = token_mixer(modulate(x, sh1, sc1), w_in[l], conv_w[l], attn_norm_g[l],
                          conv_norm_g[l], w_out[l], cos, sin)
        x = layer_norm(DEEPNORM_ALPHA * x + g1[:, None, :] * mix, ln1_g[l], ln1_b[l])
        ff = swiglu(modulate(x, sh2, sc2), w_gate[l], w_up[l], w_down[l])
        x = layer_norm(DEEPNORM_ALPHA * x + g2[:, None, :] * ff, ln2_g[l], ln2_b[l])
    return x
```

```python
import math
import os
from contextlib import ExitStack
import numpy as np
import concourse.bass as bass
import concourse.mybir as mybir
from concourse.bass_utils import run_bass_kernel_spmd

F32 = mybir.dt.float32
BF16 = mybir.dt.bfloat16
AF = mybir.ActivationFunctionType
ALU = mybir.AluOpType
AX = mybir.AxisListType

D = 1024
SEQ = 4096
NHEAD = 8
DFF = 2816
NFF = DFF // 128
ALPHA = 2.0 ** 0.25
EPS = 1e-5
TOPK = 256
NBIS = 16
NEGBIG = -30000.0

OQ, OK_, OV, OQI, OKI, OWI, OGB, OGC, OHC = 0, 512, 576, 640, 896, 928, 936, 1448, 1960


def _sw(d):
    return d + 8 if d < 8 else d - 8


def build_chunks():
    chunks = {}
    order = []

    def add(name, cols):
        cols = list(cols) + [-1] * (128 - len(cols))
        chunks[name] = cols
        order.append(name)

    for i in range(4):
        A, B = 2 * i, 2 * i + 1
        cols = [OQ + A * 64 + d for d in range(16)] + [OQ + B * 64 + d for d in range(16)]
        cols += [OQ + A * 64 + d for d in range(16, 64)] + [OQ + B * 64 + d for d in range(16, 64)]
        add(f"q{i}", cols)
        sw = [OQ + A * 64 + _sw(d) for d in range(16)] + [OQ + B * 64 + _sw(d) for d in range(16)]
        add(f"qsw{i}", sw)
    kc = [OK_ + d for d in range(16)] * 2 + [OK_ + d for d in range(16, 64)] * 2
    add("kk", kc)
    add("kksw", [OK_ + _sw(d) for d in range(16)] * 2)
    for i in range(2):
        cols, sw = [], []
        for hp in range(4):
            h = 4 * i + hp
            cols += [OQI + h * 32 + d for d in range(32)]
            sw += [OQI + h * 32 + _sw(d) for d in range(16)] + [-1] * 16
        add(f"qi{i}", cols)
        add(f"qisw{i}", sw)
    add("ki3", [OKI + d for d in range(32)] * 3)
    add("ki3sw", ([OKI + _sw(d) for d in range(16)] + [-1] * 16) * 3)
    add("vwi", [OV + d for d in range(64)] + [OWI + d for d in range(8)])
    for j in range(4):
        add(f"gc{j}", [OGC + j * 128 + d for d in range(128)])
        add(f"hc{j}", [OHC + j * 128 + d for d in range(128)])
        add(f"gb{j}", [OGB + j * 128 + d for d in range(128)])
    return chunks, order


CHUNKS, CH_ORDER = build_chunks()
CH_IDX = {n: i for i, n in enumerate(CH_ORDER)}
NCH = len(CH_ORDER)

PV_INV, PV_NEG1, PV_NSGN, PV_NEGM, PV_ONEM, PV_NSGNM, PV_MA, PV_MB = range(8)
PV_CW = 8
PV_G = 20
PV_LN1G = 28
PV_LN1B = 36
PV_BADA = 44
PV_EPS = 76
PV_ZERO = 77
PV_P2 = 78
NPV = 102


class Tracker:
    def __init__(self, nc, nds=40):
        self.nc = nc
        self.eng = {"pe": nc.tensor, "act": nc.scalar, "dve": nc.vector, "pool": nc.gpsimd, "sp": nc.sync}
        self.sem = {k: nc.alloc_semaphore("s_" + k) for k in self.eng}
        self.cnt = {k: 0 for k in self.eng}
        self.known = {k: {} for k in self.eng}
        self.res = {}
        self.dsem = [nc.alloc_semaphore(f"d{i}") for i in range(nds)]
        self.dval = [0] * nds
        self.dnext = 0
        self.nds = nds

    def _semh(self, sk):
        return self.sem[sk[1]] if sk[0] == "e" else self.dsem[sk[1]]

    def _wait(self, en, sk, val):
        if val <= 0:
            return
        if self.known[en].get(sk, 0) >= val:
            return
        self.eng[en].wait_ge(self._semh(sk), val)
        self.known[en][sk] = val

    def _deps(self, en, reads, writes, is_dma):
        for k in reads:
            r = self.res.get(k)
            if r and r["w"]:
                sk, v = r["w"]
                if (not is_dma) and sk == ("e", en) and en == "pe":
                    continue
                self._wait(en, sk, v)
        for k in writes:
            r = self.res.get(k)
            if not r:
                continue
            if r["w"]:
                sk, v = r["w"]
                if not ((not is_dma) and sk == ("e", en) and en == "pe"):
                    self._wait(en, sk, v)
            for sk, v in r["r"].items():
                if (not is_dma) and sk == ("e", en) and en == "pe":
                    continue
                self._wait(en, sk, v)

    def _commit(self, tok, reads, writes):
        sk, v = tok
        for k in reads:
            r = self.res.setdefault(k, {"w": None, "r": {}})
            r["r"][sk] = max(r["r"].get(sk, 0), v)
        for k in writes:
            self.res[k] = {"w": tok, "r": {}}

    def op(self, en, fn, reads=(), writes=()):
        self._deps(en, reads, writes, False)
        ins = fn(self.eng[en])
        if isinstance(ins, (list, tuple)):
            ins = ins[-1]
        self.cnt[en] += 1
        ins.then_inc(self.sem[en], 1)
        self._commit((("e", en), self.cnt[en]), reads, writes)

    def dma(self, en, out, in_, reads=(), writes=()):
        if en == "pool":
            self.dsem.append(self.nc.alloc_semaphore(f"dq{len(self.dsem)}"))
            self.dval.append(0)
            s = len(self.dsem) - 1
        else:
            s = self.dnext
            self.dnext = (self.dnext + 1) % self.nds
        self._wait(en, ("d", s), self.dval[s])
        self._deps(en, reads, writes, True)
        self.dval[s] += 16
        self.eng[en].dma_start(out=out, in_=in_).then_inc(self.dsem[s], 16)
        self._commit((("d", s), self.dval[s]), reads, writes)

    def barrier(self):
        for en in self.eng:
            for e2 in self.eng:
                if e2 != en:
                    self._wait(en, ("e", e2), self.cnt[e2])
            for si in range(len(self.dsem)):
                self._wait(en, ("d", si), self.dval[si])

    def wait_all(self, en):
        for k, r in self.res.items():
            if r["w"]:
                self._wait(en, *r["w"])


class Pool:
    def __init__(self, name, tiles):
        self.name, self.tiles, self.i = name, tiles, 0

    def next(self):
        i = self.i
        self.i = (self.i + 1) % len(self.tiles)
        return self.tiles[i], (self.name, i)


class _Stop(Exception):
    pass


def build_program(nb, nblk, debug=False):
    try:
        return _build_program(nb, nblk, debug)
    except _Stop as st:
        T, nc = st.args
        for en in ("sp", "act", "pool"):
            T.wait_all(en)
        return nc


def _build_program(nb, nblk, debug=False):
    STOP = os.environ.get("KSTOP", "")
    S = nblk * 512
    NT = nb * S
    nc = bass.Bass("TRN2", target_bir_lowering=False)

    def din(name, shape, dt=F32):
        return nc.dram_tensor(name, list(shape), dt, kind="ExternalInput").ap()

    x_d = din("x", [NT, D])
    cT_d = din("cT", [128, 8, nb])
    wada_d = din("w_ada", [6, 128, 8 * 1024])
    badabc_d = din("b_ada_bc", [128, 2048])
    win_d = din("w_in", [NCH, 128, 8 * 128])
    wout_d = din("w_out", [128, 8, 1024])
    wgu_d = din("w_gu", [NFF, 128, 8 * 256])
    wdn_d = din("w_down", [128, NFF, 1024])
    pv_d = din("pvec", [128, NPV])
    lnbc_d = din("ln_bc", [128, 4, 1024])
    ident_d = din("ident", [128, 128])
    negi_d = din("negi4", [128, 512])
    sel_d = din("sel", [128, 2, 4, 96])
    bones_d = din("bones", [128, 128])
    nrm_d = din("nrm", [65, 64])
    caus_d = din("causneg", [128, 128])
    pos_d = din("posrow", [128, SEQ])
    out_d = nc.dram_tensor("out", [NT, D], F32, kind="ExternalOutput").ap()

    winbf = nc.dram_tensor("winbf", [NCH, 128, 8 * 128], BF16).ap()
    wgubf = nc.dram_tensor("wgubf", [NFF, 128, 8 * 256], BF16).ap()
    ropetab = nc.dram_tensor("ropetab", [128, 4, SEQ], F32).ap()
    gbc_d = nc.dram_tensor("gbc", [2 * nb, 128, 1024], F32).ap()
    y1_d = nc.dram_tensor("y1s", [NT, D], F32).ap()
    dbg = {}
    if debug:
        for nm, shp in (("d_h1T", [128, 8 * 512]), ("d_qpad", [128, 8 * 512]), ("d_KK", [128, 512]),
                        ("d_acc", [128, 512]), ("d_thr", [128, 8]), ("d_attnT", [128, 512]),
                        ("d_conv", [128, 4 * 512]), ("d_mv", [128, 64]), ("d_qi3", [128, 8 * 512]),
                        ("d_KI3", [128, 512]), ("d_rt", [128, 4 * 512]), ("d_z", [128, 1024])):
            dbg[nm] = nc.dram_tensor(nm, shp, F32, kind="ExternalOutput").ap()

    T = Tracker(nc)
    def sb(name, shape, dt):
        return nc.alloc_sbuf_tensor("sb_" + name, shape, dt)
    pv = sb("pv", [128, NPV], F32)
    ident = sb("ident", [128, 128], F32)
    negi = sb("negi", [128, 512], BF16)
    sel = sb("sel", [128, 2, 4, 96], BF16)
    bones = sb("bones", [128, 128], F32)
    nrm = sb("nrm", [65, 64], F32)
    caus = sb("caus", [128, 128], F32)
    mv = sb("mv", [128, 4, 8, nb], F32)
    s2 = sb("s2", [128, 2, 8, nb], F32)
    psum = [nc.alloc_psum_tensor(f"ps{i}", [128, 512], F32) for i in range(8)]
    PS = Pool("ps", psum[:6])
    psO = psum[6:8]
    tmps = [sb(f"tmp{i}", [128, 512], F32) for i in range(7)]
    TP = Pool("tmp", tmps)

    def load_const(dst, src, key, eng="sp"):
        T.dma(eng, dst, src, writes=[key])

    load_const(pv[:], pv_d, "pv")
    load_const(ident[:], ident_d, "ident")
    load_const(bones[:], bones_d, "bones")
    load_const(nrm[:], nrm_d, "nrm")
    load_const(caus[:], caus_d, "caus")
    load_const(negi[:], negi_d, "negi", "pool")
    load_const(sel[:], sel_d, "sel", "pool")
    for c0 in range(0, NCH, 8):
        c1 = min(NCH, c0 + 8)
        T.dma("pool", winbf[c0:c1], win_d[c0:c1], writes=["winbf"])
    for c0 in range(0, NFF, 6):
        c1 = min(NFF, c0 + 6)
        T.dma("pool", wgubf[c0:c1], wgu_d[c0:c1], writes=["wgubf"])

    with ExitStack() as es:
        wada_sb = es.enter_context(nc.sbuf_tensor("sb_wada_sb", [128, 8, 1024], BF16))
        cTs = es.enter_context(nc.sbuf_tensor("sb_cTs", [128, 8, nb], F32))
        cact = es.enter_context(nc.sbuf_tensor("sb_cact", [128, 8, nb], BF16))
        crep = es.enter_context(nc.sbuf_tensor("sb_crep", [128, 8, nb, 128], BF16))
        bbc = es.enter_context(nc.sbuf_tensor("sb_bbc", [128, 2048], F32))
        gtmp = es.enter_context(nc.sbuf_tensor("sb_gtmp", [128, 1024], F32))
        rtab = es.enter_context(nc.sbuf_tensor("sb_rtab", [128, 4, 512], F32))
        posb = es.enter_context(nc.sbuf_tensor("sb_posb", [128, 512], F32))
        T.dma("sp", cTs[:], cT_d, writes=["cTs"])
        T.dma("sp", bbc[:], badabc_d, writes=["bbc"])
        T.op("act", lambda e: e.activation(out=cact[:], in_=cTs[:], func=AF.Silu), ["cTs"], ["cact"])
        for kt in range(8):
            for b in range(nb):
                T.op("dve", lambda e: e.tensor_copy(out=crep[:, kt, b, :],
                                                    in_=cact[:, kt, b:b + 1].to_broadcast([128, 128])),
                     ["cact"], ["crep"])
        for j in range(6):
            T.dma("pool", wada_sb[:].rearrange("p k n -> p (k n)"), wada_d[j], writes=["wada"])
            if j in (2, 5):
                gi = 0 if j == 2 else 1
                for b in range(nb):
                    for half in range(2):
                        p, pk = PS.next()
                        T.op("pe", lambda e: [e.matmul(p[:, :], lhsT=crep[:, kt, b, :],
                                                       rhs=wada_sb[:, kt, half * 512:(half + 1) * 512],
                                                       start=(kt == 0), stop=(kt == 7)) for kt in range(8)],
                             ["crep", "wada"], [pk])
                        T.op("dve", lambda e: e.tensor_tensor(
                            out=gtmp[:, half * 512:(half + 1) * 512], in0=p[:, :],
                            in1=bbc[:, gi * 1024 + half * 512: gi * 1024 + (half + 1) * 512], op=ALU.add),
                             [pk, "bbc"], ["gtmp"])
                    T.dma("sp", gbc_d[gi * nb + b], gtmp[:], reads=["gtmp"], writes=["gbc"])
            else:
                idx = {0: 0, 1: 1, 3: 2, 4: 3}[j]
                for m in range(8):
                    p, pk = PS.next()
                    T.op("pe", lambda e: [e.matmul(p[:, 0:nb], lhsT=wada_sb[:, kt, m * 128:(m + 1) * 128],
                                                   rhs=cact[:, kt, :], start=(kt == 0), stop=(kt == 7))
                                          for kt in range(8)], ["cact", "wada"], [pk])
                    T.op("dve", lambda e: e.tensor_scalar(
                        out=mv[:, idx, m, :], in0=p[:, 0:nb], scalar1=pv[:, PV_BADA + idx * 8 + m: PV_BADA + idx * 8 + m + 1],
                        scalar2=(1.0 if idx in (1, 3) else 0.0), op0=ALU.add, op1=ALU.add), [pk, "pv"], ["mv"])
        for b in range(nb):
            T.op("dve", lambda e: e.tensor_tensor(out=s2[:, 0, :, b], in0=mv[:, 3, :, b],
                                                  in1=pv[:, PV_LN1G:PV_LN1G + 8], op=ALU.mult), ["mv", "pv"], ["s2a"])
            T.op("dve", lambda e: e.tensor_tensor(out=s2[:, 1, :, b], in0=mv[:, 3, :, b],
                                                  in1=pv[:, PV_LN1B:PV_LN1B + 8], op=ALU.mult), ["mv", "pv"], ["s2b"])
            T.op("dve", lambda e: e.tensor_tensor(out=s2[:, 1, :, b], in0=s2[:, 1, :, b],
                                                  in1=mv[:, 2, :, b], op=ALU.add), ["mv", "s2b"], ["s2b"])
        for c in range(nblk):
            T.dma("sp", posb[:], pos_d[:, c * 512:(c + 1) * 512], writes=["posb"])
            a0, a0k = TP.next()
            T.op("dve", lambda e: e.tensor_scalar(out=a0[:], in0=posb[:], scalar1=pv[:, PV_INV:PV_INV + 1],
                                                  scalar2=None, op0=ALU.mult), ["posb", "pv"], [a0k])
            a1, a1k = TP.next()
            a2, a2k = TP.next()
            C1 = 6.28125
            C2 = 2 * math.pi - C1
            MAGIC = 12582912.0
            for (aa, aak, shift) in ((a1, a1k, 0.0), (a2, a2k, 0.5 * math.pi)):
                kf, kfk = TP.next()
                T.op("dve", lambda e: e.tensor_scalar(out=aa[:], in0=a0[:], scalar1=shift, scalar2=None, op0=ALU.add),
                     [a0k], [aak])
                T.op("dve", lambda e: e.tensor_scalar(out=kf[:], in0=aa[:], scalar1=1.0 / (2 * math.pi), scalar2=None,
                                                      op0=ALU.mult), [aak], [kfk])
                T.op("dve", lambda e: e.tensor_scalar(out=kf[:], in0=kf[:], scalar1=MAGIC, scalar2=None, op0=ALU.add),
                     [kfk], [kfk])
                T.op("dve", lambda e: e.tensor_scalar(out=kf[:], in0=kf[:], scalar1=-MAGIC, scalar2=None, op0=ALU.add),
                     [kfk], [kfk])
                T.op("dve", lambda e: e.scalar_tensor_tensor(out=aa[:], in0=kf[:], scalar=-C1, in1=aa[:],
                                                             op0=ALU.mult, op1=ALU.add), [kfk, aak], [aak])
                T.op("dve", lambda e: e.scalar_tensor_tensor(out=aa[:], in0=kf[:], scalar=-C2, in1=aa[:],
                                                             op0=ALU.mult, op1=ALU.add), [kfk, aak], [aak])
                T.op("dve", lambda e: e.tensor_scalar(out=aa[:], in0=aa[:], scalar1=math.pi, scalar2=-math.pi,
                                                      op0=ALU.min, op1=ALU.max), [aak], [aak])
            T.op("act", lambda e: e.activation(out=a1[:], in_=a1[:], func=AF.Sin), [a1k], [a1k])
            T.op("act", lambda e: e.activation(out=a2[:], in_=a2[:], func=AF.Sin), [a2k], [a2k])

            def col(i):
                return pv[:, i:i + 1]
            T.op("dve", lambda e: e.tensor_scalar(out=rtab[:, 0, :], in0=a2[:], scalar1=col(PV_NEG1), scalar2=None,
                                                  op0=ALU.mult), [a2k, "pv"], ["rtab"])
            T.op("dve", lambda e: e.tensor_scalar(out=rtab[:, 1, :], in0=a1[:], scalar1=col(PV_NSGN), scalar2=None,
                                                  op0=ALU.mult), [a1k, "pv"], ["rtab"])
            T.op("dve", lambda e: e.tensor_scalar(out=rtab[:, 2, :], in0=a2[:], scalar1=col(PV_NEGM),
                                                  scalar2=col(PV_ONEM), op0=ALU.mult, op1=ALU.add), [a2k, "pv"], ["rtab"])
            T.op("dve", lambda e: e.tensor_scalar(out=rtab[:, 3, :], in0=a1[:], scalar1=col(PV_NSGNM), scalar2=None,
                                                  op0=ALU.mult), [a1k, "pv"], ["rtab"])
            T.dma("sp", ropetab[:, :, c * 512:(c + 1) * 512], rtab[:], reads=["rtab"], writes=["ropetab"])

    T.barrier()
    if STOP == "setup":
        raise _Stop(T, nc)
    with ExitStack() as es:
        wout_sb = es.enter_context(nc.sbuf_tensor("sb_wout_sb", [128, 8, 1024], BF16))
        wring = es.enter_context(nc.sbuf_tensor("sb_wring", [128, 4, 8, 128], BF16))
        KK = es.enter_context(nc.sbuf_tensor("sb_KK", [128, SEQ], BF16))
        KI3 = es.enter_context(nc.sbuf_tensor("sb_KI3", [96, SEQ], BF16))
        Vaug = es.enter_context(nc.sbuf_tensor("sb_Vaug", [128, 32, 65], BF16))
        wi_sb = es.enter_context(nc.sbuf_tensor("sb_wi_sb", [128, 32, 8], F32))
        rt = es.enter_context(nc.sbuf_tensor("sb_rt", [128, 4, 512], F32))
        xb = es.enter_context(nc.sbuf_tensor("sb_xb", [128, 4, 1024], F32))
        h1T = es.enter_context(nc.sbuf_tensor("sb_h1T", [128, 8, 512], BF16))
        qpad = es.enter_context(nc.sbuf_tensor("sb_qpad", [128, 2, 2, 4, 512], BF16))
        qi3 = es.enter_context(nc.sbuf_tensor("sb_qi3", [96, 8, 512], BF16))
        convb = es.enter_context(nc.sbuf_tensor("sb_convb", [128, 2, 4, 512], BF16))
        hi_t = es.enter_context(nc.sbuf_tensor("sb_hi_t", [128, 2, 512], BF16))
        lo_t = es.enter_context(nc.sbuf_tensor("sb_lo_t", [128, 2, 512], BF16))
        klo = es.enter_context(nc.sbuf_tensor("sb_klo", [128, 512], BF16))
        ubuf = es.enter_context(nc.sbuf_tensor("sb_ubuf", [128, 514], F32))
        halo = es.enter_context(nc.sbuf_tensor("sb_halo", [128, 4, 2], F32))
        xr = es.enter_context(nc.sbuf_tensor("sb_xr", [128, 1024], F32))
        acc = es.enter_context(nc.sbuf_tensor("sb_acc", [128, SEQ], F32))
        Rc = es.enter_context(nc.sbuf_tensor("sb_Rc", [128, 2, 1024], F32))
        nmask = es.enter_context(nc.sbuf_tensor("sb_nmask", [128, SEQ], BF16))
        nmaskb = es.enter_context(nc.sbuf_tensor("sb_nmaskb", [128, SEQ], BF16))
        rngk = es.enter_context(nc.sbuf_tensor("sb_rngk", [128, 32], F32))
        PT = es.enter_context(nc.sbuf_tensor("sb_PT", [128, 2, 1024], BF16))
        attnT = es.enter_context(nc.sbuf_tensor("sb_attnT", [128, 2, 4, 128], BF16))
        zt = es.enter_context(nc.sbuf_tensor("sb_zt", [128, 1024], F32))
        yh = es.enter_context(nc.sbuf_tensor("sb_yh", [128, 2, 1024], F32))
        g1bc = es.enter_context(nc.sbuf_tensor("sb_g1bc", [128, 1024], F32))
        bis = es.enter_context(nc.sbuf_tensor("sb_bis", [128, 16], F32))
        stat = es.enter_context(nc.sbuf_tensor("sb_stat", [128, 16], F32))
        for kt in range(8):
            wstage, wsk = TP.next()
            for hf in range(2):
                T.dma("sp", wstage[:], wout_d[:, kt, hf * 512:(hf + 1) * 512], writes=[wsk])
                T.op("dve", lambda e: e.tensor_scalar(out=wout_sb[:, kt, hf * 512:(hf + 1) * 512], in0=wstage[:],
                                                      scalar1=pv[:, PV_G + kt:PV_G + kt + 1], scalar2=None, op0=ALU.mult),
                     [wsk, "pv"], ["wout"])
        T.op("pool", lambda e: e.memset(Vaug[:], 1.0), [], ["Vaug"])
        T.op("dve", lambda e: e.memset(bis[:], 0.0), [], ["thr", "bmn", "bmx", "brng", "cand", "cnt", "bt"])

        ring_i = [0]

        def wload(name):
            ci = CH_IDX[name]
            s = ring_i[0]
            ring_i[0] = (s + 1) % 4
            key = ("wring", s)
            T.dma("sp", wring[:, s, :, :].rearrange("p k n -> p (k n)"), winbf[ci], reads=["winbf"], writes=[key])
            return wring[:, s, :, :], key

        def proj(name, M):
            w, wk = wload(name)
            p, pk = PS.next()
            T.op("pe", lambda e: [e.matmul(p[0:M, :], lhsT=w[:, kt, 0:M], rhs=h1T[:, kt, :],
                                           start=(kt == 0), stop=(kt == 7)) for kt in range(8)],
                 [wk, "h1T"], [pk])
            return p, pk

        def rope(p, pk, psw, pswk, rows, ci, si):
            t1, t1k = TP.next()
            t2, t2k = TP.next()
            T.op("act", lambda e: e.activation(out=t1[0:rows, :], in_=p[0:rows, :], func=AF.Copy), [pk], [t1k])
            T.op("act", lambda e: e.activation(out=t2[0:rows, :], in_=psw[0:rows, :], func=AF.Copy), [pswk], [t2k])
            T.op("dve", lambda e: e.tensor_tensor(out=t1[0:rows, :], in0=t1[0:rows, :], in1=rt[0:rows, ci, :],
                                                  op=ALU.mult), [t1k, "rt"], [t1k])
            T.op("dve", lambda e: e.tensor_tensor(out=t2[0:rows, :], in0=t2[0:rows, :], in1=rt[0:rows, si, :],
                                                  op=ALU.mult), [t2k, "rt"], [t2k])
            T.op("dve", lambda e: e.tensor_tensor(out=t1[0:rows, :], in0=t1[0:rows, :], in1=t2[0:rows, :],
                                                  op=ALU.add), [t1k, t2k], [t1k])
            return t1, t1k

        def phaseA(b, Bb, part):
            bp = Bb % 2
            T0 = b * S + Bb * 512
            P0 = Bb * 512
            if part == 1:
                T.dma("sp", rt[:], ropetab[:, :, P0:P0 + 512], reads=["ropetab"], writes=["rt"])
                for tt in range(4):
                    T.dma("sp", xb[:, tt, :], x_d[T0 + tt * 128:T0 + (tt + 1) * 128, :], writes=[("xb", tt)])
                xkeys = [("xb", tt) for tt in range(4)]
                for kt in range(8):
                    p, pk = PS.next()
                    T.op("pe", lambda e: [e.transpose(out=p[:, tt * 128:(tt + 1) * 128],
                                                      in_=xb[:, tt, kt * 128:(kt + 1) * 128], identity=ident[:])
                                          for tt in range(4)], xkeys + ["ident"], [pk])
                    T.op("act", lambda e: e.activation(out=h1T[:, kt, :], in_=p[:, :], func=AF.Identity,
                                                       scale=mv[:, 1, kt, b:b + 1], bias=mv[:, 0, kt, b:b + 1]),
                         [pk, "mv"], ["h1T"])
                p, pk = proj("kk", 128)
                psw, pswk = proj("kksw", 32)
                t1, t1k = rope(p, pk, psw, pswk, 32, 0, 1)
                T.op("act", lambda e: e.activation(out=KK[:, P0:P0 + 512], in_=p[:, :], func=AF.Copy), [pk], ["KK"])
                T.op("dve", lambda e: e.tensor_copy(out=KK[0:32, P0:P0 + 512], in_=t1[0:32, :]), [t1k], ["KK"])
                w, wk = wload("vwi")
                for tt in range(4):
                    p, pk = PS.next()
                    T.op("pe", lambda e: [e.matmul(p[:, 0:64], lhsT=h1T[:, kt, tt * 128:(tt + 1) * 128], rhs=w[:, kt, 0:64],
                                                   start=(kt == 0), stop=(kt == 7)) for kt in range(8)] +
                                         [e.matmul(p[:, 64:72], lhsT=h1T[:, kt, tt * 128:(tt + 1) * 128], rhs=w[:, kt, 64:72],
                                                   start=(kt == 0), stop=(kt == 7)) for kt in range(8)],
                         [wk, "h1T"], [pk])
                    kb = Bb * 4 + tt
                    T.op("act", lambda e: e.activation(out=Vaug[:, kb, 0:64], in_=p[:, 0:64], func=AF.Copy), [pk], ["Vaug"])
                    T.op("dve", lambda e: e.tensor_scalar(out=wi_sb[:, kb, :], in0=p[:, 64:72], scalar1=0.0625, scalar2=None,
                                                          op0=ALU.mult), [pk, "Vaug"], ["wi"])
                for j in range(4):
                    uk = "u"
                    hk_ = ("halo", j)
                    if Bb == 0:
                        T.op("pool", lambda e: e.memset(ubuf[:, 0:2], 0.0), [], [uk])
                    else:
                        T.op("pool", lambda e: e.tensor_copy(out=ubuf[:, 0:2], in_=halo[:, j, :]), [hk_], [uk])
                    pgc, pgck = proj(f"gc{j}", 128)
                    phc, phck = proj(f"hc{j}", 128)
                    pgb, pgbk = proj(f"gb{j}", 128)
                    g, gk = TP.next()
                    T.op("act", lambda e: e.activation(out=g[:], in_=pgc[:, :], func=AF.Copy), [pgck], [gk])
                    T.op("dve", lambda e: e.tensor_tensor(out=ubuf[:, 2:514], in0=phc[:, :], in1=g[:], op=ALU.mult),
                         [phck, gk], [uk])
                    c0, c0k = TP.next()
                    cw = PV_CW + j * 3
                    T.op("act", lambda e: e.activation(out=c0[:], in_=ubuf[:, 2:514], func=AF.Identity,
                                                       scale=pv[:, cw + 2:cw + 3], bias=pv[:, PV_ZERO:PV_ZERO + 1]), [uk, "pv"], [c0k])
                    T.op("dve", lambda e: e.scalar_tensor_tensor(out=c0[:], in0=ubuf[:, 1:513], scalar=pv[:, cw + 1:cw + 2],
                                                                 in1=c0[:], op0=ALU.mult, op1=ALU.add), [uk, c0k, "pv"], [c0k])
                    T.op("dve", lambda e: e.scalar_tensor_tensor(out=c0[:], in0=ubuf[:, 0:512], scalar=pv[:, cw:cw + 1],
                                                                 in1=c0[:], op0=ALU.mult, op1=ALU.add), [uk, c0k, "pv"], [c0k])
                    T.op("dve", lambda e: e.tensor_tensor(out=c0[:], in0=pgb[:, :], in1=c0[:], op=ALU.mult), [pgbk, c0k], [c0k])
                    ysq, ysqk = TP.next()
                    T.op("act", lambda e: e.activation(out=ysq[:], in_=c0[:], func=AF.Square), [c0k], [ysqk])
                    pss, pssk = PS.next()
                    T.op("pe", lambda e: e.matmul(pss[:, :], lhsT=bones[:], rhs=ysq[:], start=True, stop=True),
                         ["bones", ysqk], [pssk])
                    r, rk = TP.next()
                    T.op("act", lambda e: e.activation(out=r[:], in_=pss[:, :], func=AF.Ln, bias=pv[:, PV_EPS:PV_EPS + 1]),
                         [pssk, "pv"], [rk])
                    T.op("act", lambda e: e.activation(out=r[:], in_=r[:], func=AF.Exp, scale=-0.5), [rk], [rk])
                    T.op("dve", lambda e: e.tensor_tensor(out=convb[:, bp, j, :], in0=c0[:], in1=r[:], op=ALU.mult),
                         [c0k, rk], [("convb", bp)])
                    T.op("pool", lambda e: e.tensor_copy(out=halo[:, j, :], in_=ubuf[:, 512:514]), [uk], [hk_])

            if part == 2:
                for i in range(4):
                    p, pk = proj(f"q{i}", 128)
                    psw, pswk = proj(f"qsw{i}", 32)
                    t1, t1k = rope(p, pk, psw, pswk, 32, 0, 1)
                    T.op("act", lambda e: [e.activation(out=qpad[:, bp, 0, i, :], in_=p[:, :], func=AF.Identity,
                                                        scale=pv[:, PV_MA:PV_MA + 1], bias=pv[:, PV_ZERO:PV_ZERO + 1]),
                                           e.activation(out=qpad[:, bp, 1, i, :], in_=p[:, :], func=AF.Identity,
                                                        scale=pv[:, PV_MB:PV_MB + 1], bias=pv[:, PV_ZERO:PV_ZERO + 1])], [pk, "pv"], [("qpad", bp)])
                    T.op("dve", lambda e: [e.tensor_scalar(out=qpad[0:32, bp, 0, i, :], in0=t1[0:32, :],
                                                           scalar1=pv[0:32, PV_MA:PV_MA + 1], scalar2=None, op0=ALU.mult),
                                           e.tensor_scalar(out=qpad[0:32, bp, 1, i, :], in0=t1[0:32, :],
                                                           scalar1=pv[0:32, PV_MB:PV_MB + 1], scalar2=None, op0=ALU.mult)],
                         [t1k, "pv"], [("qpad", bp)])
                for i in range(2):
                    p, pk = proj(f"qi{i}", 128)
                    psw, pswk = proj(f"qisw{i}", 128)
                    t1, t1k = rope(p, pk, psw, pswk, 128, 2, 3)
                    T.op("act", lambda e: e.activation(out=hi_t[:, i, :], in_=t1[:, :], func=AF.Copy), [t1k], [("hi", i)])
                    T.op("dve", lambda e: e.tensor_tensor(out=lo_t[:, i, :], in0=t1[:, :], in1=hi_t[:, i, :],
                                                          op=ALU.subtract), [t1k, ("hi", i)], [("lo", i)])
                for h in range(8):
                    i, hp = h // 4, h % 4
                    p, pk = PS.next()
                    T.op("pe", lambda e: [e.matmul(p[0:96, :], lhsT=sel[:, 0, hp, :], rhs=hi_t[:, i, :], start=True, stop=False),
                                          e.matmul(p[0:96, :], lhsT=sel[:, 1, hp, :], rhs=lo_t[:, i, :], start=False, stop=True)],
                         ["sel", ("hi", i), ("lo", i)], [pk])
                    T.op("act", lambda e: e.activation(out=qi3[0:96, h, :], in_=p[0:96, :], func=AF.Copy), [pk], ["qi3"])
                p, pk = proj("ki3", 96)
                psw, pswk = proj("ki3sw", 96)
                t1, t1k = rope(p, pk, psw, pswk, 96, 2, 3)
                T.op("act", lambda e: e.activation(out=KI3[0:96, P0:P0 + 512], in_=t1[0:96, :], func=AF.Copy), [t1k], ["KI3"])
                T.op("dve", lambda e: e.tensor_tensor(out=klo[64:96, :], in0=t1[64:96, :], in1=KI3[64:96, P0:P0 + 512],
                                                      op=ALU.subtract), [t1k, "KI3"], ["klo"])
                T.op("dve", lambda e: e.tensor_copy(out=KI3[64:96, P0:P0 + 512], in_=klo[64:96, :]), ["klo"], ["KI3"])

        PS_idx = Pool("ps", psum[0:2])
        PS_att = Pool("ps", psum[2:4])
        PS_att.off = 2
        PS_misc = Pool("ps", psum[4:6])
        PS_misc.off = 4
        nmask2 = [nmask, nmaskb]

        def phF(b, Bb, qq):
            qb = Bb * 4 + qq
            Sk = (qb + 1) * 128
            qs = slice(qq * 128, (qq + 1) * 128)
            nm = nmask2[qb % 2]
            nmk = ("nmask", qb % 2)
            nch = (Sk + 1023) // 1024
            for h in range(8):
                for c in range(nch):
                    c0_ = c * 1024
                    wd = min(1024, Sk - c0_)
                    rcs = (h * nch + c) % 2
                    rk = ("Rc", rcs)
                    for sub in range((wd + 511) // 512):
                        s0 = c0_ + sub * 512
                        w2 = min(512, Sk - s0)
                        p, pk = PS_idx.next()
                        T.op("pe", lambda e: e.matmul(p[:, 0:w2], lhsT=qi3[0:96, h, qs], rhs=KI3[0:96, s0:s0 + w2],
                                                      start=True, stop=True), ["qi3", "KI3"], [pk])
                        T.op("act", lambda e: e.activation(out=Rc[:, rcs, sub * 512:sub * 512 + w2], in_=p[:, 0:w2],
                                                           func=AF.Relu), [pk], [rk])
                    if h == 0:
                        T.op("dve", lambda e: e.tensor_scalar(out=acc[:, c0_:c0_ + wd], in0=Rc[:, rcs, 0:wd],
                                                              scalar1=wi_sb[:, qb, 0:1], scalar2=None, op0=ALU.mult),
                             [rk, "wi"], ["acc"])
                    else:
                        T.op("dve", lambda e: e.scalar_tensor_tensor(out=acc[:, c0_:c0_ + wd], in0=Rc[:, rcs, 0:wd],
                                                                     scalar=wi_sb[:, qb, h:h + 1], in1=acc[:, c0_:c0_ + wd],
                                                                     op0=ALU.mult, op1=ALU.add), [rk, "wi", "acc"], ["acc"])
            thr = bis[:, 0:1]
            if Sk <= TOPK:
                T.op("dve", lambda e: e.tensor_tensor(out=acc[:, Sk - 128:Sk], in0=acc[:, Sk - 128:Sk], in1=caus[:],
                                                      op=ALU.add), ["acc", "caus"], ["acc"])
                T.op("dve", lambda e: e.memset(thr, -1.0e29), [], ["thr"])
            else:
                T.op("dve", lambda e: [e.tensor_reduce(out=bis[:, 1:2], in_=acc[:, 0:Sk], axis=AX.X, op=ALU.min),
                                       e.tensor_reduce(out=bis[:, 2:3], in_=acc[:, 0:Sk], axis=AX.X, op=ALU.max)],
                     ["acc"], ["bmnx"])
                T.op("dve", lambda e: e.tensor_tensor(out=acc[:, Sk - 128:Sk], in0=acc[:, Sk - 128:Sk], in1=caus[:],
                                                      op=ALU.add), ["acc", "caus", "bmnx"], ["acc"])
                T.op("dve", lambda e: e.tensor_tensor(out=bis[:, 3:4], in0=bis[:, 2:3], in1=bis[:, 1:2], op=ALU.subtract),
                     ["bmnx"], ["brng"])
                T.op("dve", lambda e: e.tensor_scalar(out=bis[:, 3:4], in0=bis[:, 3:4], scalar1=1.001, scalar2=1e-6,
                                                      op0=ALU.mult, op1=ALU.add), ["brng"], ["brng"])
                T.op("dve", lambda e: [e.tensor_scalar(out=rngk[:, 0:NBIS + 2], in0=pv[:, PV_P2:PV_P2 + NBIS + 2],
                                                       scalar1=bis[:, 3:4], scalar2=None, op0=ALU.mult),
                                       e.scalar_tensor_tensor(out=bis[:, 4:5], in0=bis[:, 3:4], scalar=0.5, in1=bis[:, 1:2],
                                                              op0=ALU.mult, op1=ALU.add)],
                     ["brng", "bmnx", "pv"], ["rngk", "cand"])
                for it in range(1, NBIS + 1):
                    T.op("dve", lambda e: e.tensor_scalar(out=nm[:, 0:Sk], in0=acc[:, 0:Sk], scalar1=bis[:, 4:5],
                                                          scalar2=None, op0=ALU.is_ge, op1=ALU.add, accum_out=bis[:, 5:6]),
                         ["acc", "cand"], [nmk, "cnt"])
                    T.op("dve", lambda e: e.tensor_scalar(out=bis[:, 6:7], in0=bis[:, 5:6], scalar1=TOPK - 0.5, scalar2=0.5,
                                                          op0=ALU.is_ge, op1=ALU.subtract), ["cnt"], ["bt"])
                    T.op("dve", lambda e: e.scalar_tensor_tensor(out=bis[:, 4:5], in0=bis[:, 6:7], scalar=rngk[:, it:it + 1],
                                                                 in1=bis[:, 4:5], op0=ALU.mult, op1=ALU.add),
                         ["bt", "rngk", "cand"], ["cand"])
                T.op("dve", lambda e: e.tensor_tensor(out=thr, in0=bis[:, 4:5], in1=rngk[:, NBIS + 1:NBIS + 2], op=ALU.subtract),
                     ["cand", "rngk"], ["thr"])
            T.op("dve", lambda e: e.tensor_scalar(out=nm[:, 0:Sk], in0=acc[:, 0:Sk], scalar1=thr, scalar2=None,
                                                  op0=ALU.is_lt), ["acc", "thr"], [nmk])

        def phB1(b, Bb, qq):
            qb = Bb * 4 + qq
            bp = Bb % 2
            qs = slice(qq * 128, (qq + 1) * 128)
            nm = nmask2[qb % 2]
            nmk = ("nmask", qb % 2)
            for kb in range(qb + 1):
                ks = slice(kb * 128, (kb + 1) * 128)
                pts = kb % 2
                ptk = ("PT", pts)
                for par in range(2):
                    p, pk = PS_att.next()
                    pk = ("ps", pk[1] + 2)
                    T.op("pe", lambda e: [e.matmul(p[:, :].rearrange("p (a t) -> p a t", a=4), lhsT=KK[:, ks],
                                                   rhs=qpad[:, bp, par, :, qs], start=True, stop=False),
                                          e.matmul(p[:, :], lhsT=nm[:, ks], rhs=negi[:], start=False, stop=True)],
                         ["KK", ("qpad", bp), nmk, "negi"], [pk])
                    T.op("act", lambda e: e.activation(out=PT[:, pts, par * 512:(par + 1) * 512], in_=p[:, :], func=AF.Exp,
                                                       scale=0.125), [pk], [ptk])
                T.op("pe", lambda e: [e.matmul(psO[par][0:65, :], lhsT=Vaug[:, kb, :], rhs=PT[:, pts, par * 512:(par + 1) * 512],
                                               start=(kb == 0), stop=(kb == qb)) for par in range(2)],
                     [ptk, "Vaug"], ["psO"])
            for par in range(2):
                sq, sqk = TP.next()
                T.op("act", lambda e: e.activation(out=sq[0:65, :], in_=psO[par][0:65, :], func=AF.Square), ["psO"], [sqk])
                pn, pnk = PS_misc.next()
                pnk = ("ps", pnk[1] + 4)
                T.op("pe", lambda e: e.matmul(pn[0:64, :], lhsT=nrm[:, :], rhs=sq[0:65, :], start=True, stop=True),
                     ["nrm", sqk], [pnk])
                r, rk2 = TP.next()
                T.op("act", lambda e: e.activation(out=r[0:64, :], in_=pn[0:64, :], func=AF.Ln), [pnk], [rk2])
                T.op("act", lambda e: e.activation(out=r[0:64, :], in_=r[0:64, :], func=AF.Exp, scale=-0.5), [rk2], [rk2])
                T.op("dve", lambda e: e.tensor_tensor(out=attnT[par * 64:(par + 1) * 64, qb % 2, :, :].rearrange("p a t -> p (a t)"),
                                                      in0=psO[par][0:64, :], in1=r[0:64, :], op=ALU.mult),
                     ["psO", rk2], [("attnT", qb % 2)])

        def phB2(b, Bb, qq):
            qb = Bb * 4 + qq
            bp = Bb % 2
            T0 = b * S + qb * 128
            qs = slice(qq * 128, (qq + 1) * 128)
            T.dma("sp", xr[:], x_d[T0:T0 + 128, :], writes=["xr"])
            for half in range(2):
                hs = slice(half * 512, (half + 1) * 512)
                p, pk = PS_misc.next()
                pk = ("ps", pk[1] + 4)
                T.op("pe", lambda e: [e.matmul(p[:, :], lhsT=(attnT[:, qb % 2, kt, :] if kt < 4 else convb[:, bp, kt - 4, qs]),
                                               rhs=wout_sb[:, kt, hs], start=(kt == 0), stop=(kt == 7)) for kt in range(8)],
                     [("attnT", qb % 2), ("convb", bp), "wout"], [pk])
                T.op("dve", lambda e: e.tensor_tensor(out=zt[:, hs], in0=p[:, :], in1=g1bc[:, hs], op=ALU.mult),
                     [pk, "g1bc"], ["zt"])
            T.op("dve", lambda e: e.scalar_tensor_tensor(out=zt[:], in0=xr[:], scalar=ALPHA, in1=zt[:],
                                                         op0=ALU.mult, op1=ALU.add), ["xr", "zt"], ["zt"])
            ys = qb % 2
            layer_norm(zt, "zt", yh[:, ys, :], ("yh", ys))
            T.dma("sp", y1_d[T0:T0 + 128, :], yh[:, ys, :], reads=[("yh", ys)], writes=[("y1s", b, qb)])

        def layer_norm(src, srck, dst, dstk):
            T.op("dve", lambda e: [e.bn_stats(out=stat[:, 0:6], in_=src[:, 0:512]),
                                   e.bn_stats(out=stat[:, 6:12], in_=src[:, 512:1024])], [srck], ["bnst"])
            T.op("dve", lambda e: e.bn_aggr(out=stat[:, 12:14], in_=stat[:, 0:12]), ["bnst"], ["bnag"])
            T.op("act", lambda e: e.activation(out=stat[:, 14:15], in_=stat[:, 13:14], func=AF.Ln,
                                               bias=pv[:, PV_EPS:PV_EPS + 1]), ["bnag", "pv"], ["rstd"])
            T.op("act", lambda e: e.activation(out=stat[:, 14:15], in_=stat[:, 14:15], func=AF.Exp, scale=-0.5),
                 ["rstd"], ["rstd"])
            T.op("dve", lambda e: e.tensor_scalar(out=dst, in0=src[:], scalar1=stat[:, 12:13], scalar2=stat[:, 14:15],
                                                  op0=ALU.subtract, op1=ALU.mult), [srck, "bnag", "rstd"], [dstk])

        for b in range(nb):
            T.dma("sp", g1bc[:], gbc_d[0 * nb + b], reads=["gbc"], writes=["g1bc"])
            NQ = nblk * 4
            phaseA(b, 0, 1)
            phaseA(b, 0, 2)
            for q in range(NQ + 2):
                if q < NQ:
                    phF(b, q // 4, q % 4)
                    if q % 4 == 2 and q + 2 < NQ:
                        phaseA(b, (q + 2) // 4, 1)
                    if q % 4 == 3 and q + 1 < NQ:
                        phaseA(b, (q + 1) // 4, 2)
                if 0 <= q - 1 < NQ:
                    phB1(b, (q - 1) // 4, (q - 1) % 4)
                if 0 <= q - 2 < NQ:
                    phB2(b, (q - 2) // 4, (q - 2) % 4)

    T.barrier()
    if STOP == "s1":
        raise _Stop(T, nc)
    with ExitStack() as es:
        wdn_sb = es.enter_context(nc.sbuf_tensor("sb_wdn_sb", [128, NFF, 1024], BF16))
        gring = es.enter_context(nc.sbuf_tensor("sb_gring", [128, 4, 8, 256], BF16))
        yb = es.enter_context(nc.sbuf_tensor("sb_yb", [128, 2, 4, 1024], F32))
        h2T = es.enter_context(nc.sbuf_tensor("sb_h2T", [128, 2, 8, 512], BF16))
        actT = es.enter_context(nc.sbuf_tensor("sb_actT", [128, NFF, 512], BF16))
        lnbc = es.enter_context(nc.sbuf_tensor("sb_lnbc", [128, 4, 1024], F32))
        g2bc = es.enter_context(nc.sbuf_tensor("sb_g2bc", [128, 2, 1024], F32))
        z2 = es.enter_context(nc.sbuf_tensor("sb_z2", [128, 1024], F32))
        y1t = es.enter_context(nc.sbuf_tensor("sb_y1t", [128, 1024], F32))
        ot = es.enter_context(nc.sbuf_tensor("sb_ot", [128, 2, 1024], F32))
        stat = es.enter_context(nc.sbuf_tensor("sb_stat2", [128, 16], F32))
        T.dma("pool", wdn_sb[:], wdn_d, writes=["wdn"])
        T.dma("sp", lnbc[:], lnbc_d, writes=["lnbc"])
        gi = [0]

        def layer_norm2(src, srck, dst, dstk):
            T.op("dve", lambda e: [e.bn_stats(out=stat[:, 0:6], in_=src[:, 0:512]),
                                   e.bn_stats(out=stat[:, 6:12], in_=src[:, 512:1024])], [srck], ["bnst"])
            T.op("dve", lambda e: e.bn_aggr(out=stat[:, 12:14], in_=stat[:, 0:12]), ["bnst"], ["bnag"])
            T.op("act", lambda e: e.activation(out=stat[:, 14:15], in_=stat[:, 13:14], func=AF.Ln,
                                               bias=pv[:, PV_EPS:PV_EPS + 1]), ["bnag", "pv"], ["rstd"])
            T.op("act", lambda e: e.activation(out=stat[:, 14:15], in_=stat[:, 14:15], func=AF.Exp, scale=-0.5),
                 ["rstd"], ["rstd"])
            T.op("dve", lambda e: e.tensor_scalar(out=dst, in0=src[:], scalar1=stat[:, 12:13], scalar2=stat[:, 14:15],
                                                  op0=ALU.subtract, op1=ALU.mult), [srck, "bnag", "rstd"], [dstk])

        blocks = [(b, Bb) for b in range(nb) for Bb in range(nblk)]
        for b in range(nb):
            T.dma("sp", g2bc[:, b, :], gbc_d[1 * nb + b], reads=["gbc"], writes=[("g2bc", b)])

        def load_y(i):
            b, Bb = blocks[i]
            T0 = b * S + Bb * 512
            for tt in range(4):
                T.dma("sp", yb[:, i % 2, tt, :], y1_d[T0 + tt * 128:T0 + (tt + 1) * 128, :],
                      reads=[("y1s", b, Bb * 4 + tt)], writes=[("yb", i % 2, tt)])

        def transp(i):
            b, Bb = blocks[i]
            ykeys = [("yb", i % 2, tt) for tt in range(4)]
            for kt in range(8):
                p, pk = PS.next()
                T.op("pe", lambda e: [e.transpose(out=p[:, tt * 128:(tt + 1) * 128],
                                                  in_=yb[:, i % 2, tt, kt * 128:(kt + 1) * 128], identity=ident[:])
                                      for tt in range(4)], ykeys + ["ident"], [pk])
                T.op("act", lambda e: e.activation(out=h2T[:, i % 2, kt, :], in_=p[:, :], func=AF.Identity,
                                                   scale=s2[:, 0, kt, b:b + 1], bias=s2[:, 1, kt, b:b + 1]),
                     [pk, "s2a", "s2b"], [("h2T", i % 2)])

        load_y(0)
        transp(0)
        if len(blocks) > 1:
            load_y(1)
        for i, (b, Bb) in enumerate(blocks):
            T0 = b * S + Bb * 512
            hk = ("h2T", i % 2)
            for m in range(NFF):
                if m == 8 and i >= 1 and i + 1 < len(blocks):
                    load_y(i + 1)
                s_ = gi[0]
                gi[0] = (s_ + 1) % 4
                gk = ("gring", s_)
                T.dma("sp", gring[:, s_, :, :].rearrange("p k n -> p (k n)"), wgubf[m], reads=["wgubf"], writes=[gk])
                pg, pgk = PS.next()
                pu, puk = PS.next()
                T.op("pe", lambda e: [e.matmul(pg[:, :], lhsT=gring[:, s_, kt, 0:128], rhs=h2T[:, i % 2, kt, :],
                                               start=(kt == 0), stop=(kt == 7)) for kt in range(8)] +
                                     [e.matmul(pu[:, :], lhsT=gring[:, s_, kt, 128:256], rhs=h2T[:, i % 2, kt, :],
                                               start=(kt == 0), stop=(kt == 7)) for kt in range(8)],
                     [gk, hk], [pgk, puk])
                sg, sgk = TP.next()
                T.op("act", lambda e: e.activation(out=sg[:], in_=pg[:, :], func=AF.Silu), [pgk], [sgk])
                T.op("dve", lambda e: e.tensor_tensor(out=actT[:, m, :], in0=pu[:, :], in1=sg[:], op=ALU.mult),
                     [puk, sgk], ["actT"])
            if i + 1 < len(blocks):
                transp(i + 1)
            for tt in range(4):
                ts_ = slice(tt * 128, (tt + 1) * 128)
                for half in range(2):
                    hs = slice(half * 512, (half + 1) * 512)
                    p, pk = PS.next()
                    T.op("pe", lambda e: [e.matmul(p[:, :], lhsT=actT[:, m, ts_], rhs=wdn_sb[:, m, hs],
                                                   start=(m == 0), stop=(m == NFF - 1)) for m in range(NFF)],
                         ["actT", "wdn"], [pk])
                    T.op("dve", lambda e: e.tensor_tensor(out=z2[:, hs], in0=p[:, :], in1=g2bc[:, b, hs], op=ALU.mult),
                         [pk, ("g2bc", b)], ["z2"])
                T.op("pool", lambda e: e.tensor_tensor(out=y1t[:], in0=yb[:, i % 2, tt, :], in1=lnbc[:, 0, :], op=ALU.mult),
                     [("yb", i % 2, tt), "lnbc"], ["y1t"])
                T.op("pool", lambda e: e.tensor_tensor(out=y1t[:], in0=y1t[:], in1=lnbc[:, 1, :], op=ALU.add),
                     ["y1t", "lnbc"], ["y1t"])
                T.op("dve", lambda e: e.scalar_tensor_tensor(out=z2[:], in0=y1t[:], scalar=ALPHA, in1=z2[:],
                                                             op0=ALU.mult, op1=ALU.add), ["y1t", "z2"], ["z2"])
                os_ = tt % 2
                ok = ("ot", os_)
                layer_norm2(z2, "z2", ot[:, os_, :], ok)
                T.op("pool", lambda e: e.tensor_tensor(out=ot[:, os_, :], in0=ot[:, os_, :], in1=lnbc[:, 2, :], op=ALU.mult),
                     [ok, "lnbc"], [ok])
                T.op("pool", lambda e: e.tensor_tensor(out=ot[:, os_, :], in0=ot[:, os_, :], in1=lnbc[:, 3, :], op=ALU.add),
                     [ok, "lnbc"], [ok])
                T.dma("sp", out_d[T0 + tt * 128:T0 + (tt + 1) * 128, :], ot[:, os_, :], reads=[ok], writes=["out"])
    T.wait_all("sp")
    return nc


def _consts():
    ident = np.eye(128, dtype=np.float32)
    negi4 = np.tile(np.eye(128, dtype=np.float32) * np.float32(NEGBIG), (1, 4))
    sel = np.zeros((128, 2, 4, 96), np.float32)
    for hp in range(4):
        for d in range(32):
            sel[hp * 32 + d, 0, hp, d] = 1.0
            sel[hp * 32 + d, 0, hp, 64 + d] = 1.0
            sel[hp * 32 + d, 1, hp, 32 + d] = 1.0
    bones = np.zeros((128, 128), np.float32)
    bones[:64, :64] = 1.0 / 64
    bones[64:, 64:] = 1.0 / 64
    nrm = np.full((65, 64), 1.0 / 64, np.float32)
    nrm[64, :] = EPS
    t = np.arange(128)
    caus = np.where(t[None, :] <= t[:, None], 0.0, -1.0e30).astype(np.float32)
    pos = np.tile(np.arange(SEQ, dtype=np.float32)[None, :], (128, 1))
    return ident, negi4, sel, bones, nrm, caus, pos


def _pvec_static():
    pvc = np.zeros((128, NPV), np.float32)
    p = np.arange(128)
    inv = (np.float32(500000.0) ** (-(np.arange(0, 16, 2, dtype=np.float32) / np.float32(16)))).astype(np.float32)
    pvc[:, PV_INV] = inv[p % 8]
    pvc[:, PV_NEG1] = 1.0
    sgn = np.where(p % 16 < 8, -1.0, 1.0)
    m = (p % 32 < 16).astype(np.float32)
    pvc[:, PV_NSGN] = sgn
    pvc[:, PV_NEGM] = m
    pvc[:, PV_ONEM] = 1.0 - m
    pvc[:, PV_NSGNM] = sgn * m
    ma = ((p < 16) | ((p >= 32) & (p < 80))).astype(np.float32)
    pvc[:, PV_MA] = ma
    pvc[:, PV_MB] = 1.0 - ma
    pvc[:, PV_EPS] = EPS
    for k in range(24):
        pvc[:, PV_P2 + k] = 2.0 ** (-k)
    return pvc


def make_inputs(x, c, w_ada, b_ada, w_in, conv_w, attn_norm_g, conv_norm_g, w_out, ln1_g, ln1_b,
                w_gate, w_up, w_down, ln2_g, ln2_b, nb, ncores):
    f = np.float32
    x = np.asarray(x, f)
    c = np.asarray(c, f)
    ident, negi4, sel, bones, nrm, caus, pos = _consts()
    wa = np.asarray(w_ada[0], f)
    wada = np.ascontiguousarray(wa.reshape(8, 128, 6, 1024).transpose(2, 1, 0, 3)).reshape(6, 128, 8 * 1024)
    ba = np.asarray(b_ada[0], f)
    bbc = np.ascontiguousarray(np.broadcast_to(np.concatenate([ba[2048:3072], ba[5120:6144]])[None, :], (128, 2048)))
    wi = np.asarray(w_in[0], f)
    wi_pad = np.concatenate([wi, np.zeros((1024, 1), f)], axis=1)
    win = np.zeros((NCH, 128, 8, 128), f)
    for ci, name in enumerate(CH_ORDER):
        cols = np.array(CHUNKS[name])
        win[ci] = wi_pad[:, cols].reshape(8, 128, 128).transpose(1, 0, 2)
    win = win.reshape(NCH, 128, 8 * 128)
    wo = np.ascontiguousarray(np.asarray(w_out[0], f).reshape(8, 128, 1024).transpose(1, 0, 2))
    wg = np.asarray(w_gate[0], f).reshape(8, 128, NFF, 128)
    wu = np.asarray(w_up[0], f).reshape(8, 128, NFF, 128)
    wgu = np.concatenate([wg, wu], axis=3).transpose(2, 1, 0, 3)
    wgu = np.ascontiguousarray(wgu).reshape(NFF, 128, 8 * 256)
    wdn = np.ascontiguousarray(np.asarray(w_down[0], f).reshape(NFF, 128, 1024).transpose(1, 0, 2))
    pvc = _pvec_static()
    cw = np.asarray(conv_w[0], f)
    for j in range(4):
        for tap in range(3):
            pvc[:, PV_CW + j * 3 + tap] = cw[tap, j * 128:(j + 1) * 128]
    ag = np.asarray(attn_norm_g[0], f)
    cg = np.asarray(conv_norm_g[0], f)
    for kt in range(4):
        pvc[:, PV_G + kt] = ag[kt * 128:(kt + 1) * 128]
        pvc[:, PV_G + 4 + kt] = cg[kt * 128:(kt + 1) * 128]
    pvc[:, PV_LN1G:PV_LN1G + 8] = np.asarray(ln1_g[0], f).reshape(8, 128).T
    pvc[:, PV_LN1B:PV_LN1B + 8] = np.asarray(ln1_b[0], f).reshape(8, 128).T
    for idx, j in enumerate((0, 1, 3, 4)):
        pvc[:, PV_BADA + idx * 8:PV_BADA + idx * 8 + 8] = ba[j * 1024:(j + 1) * 1024].reshape(8, 128).T
    lnbc = np.stack([np.asarray(v[0], f) for v in (ln1_g, ln1_b, ln2_g, ln2_b)])
    lnbc = np.ascontiguousarray(np.broadcast_to(lnbc[None], (128, 4, 1024)))
    shared = {"w_ada": wada, "b_ada_bc": bbc, "w_in": win, "w_out": wo, "w_gu": wgu, "w_down": wdn, "pvec": pvc,
              "ln_bc": lnbc, "ident": ident, "negi4": negi4, "sel": sel, "bones": bones, "nrm": nrm,
              "causneg": caus, "posrow": pos}
    maps = []
    S = x.shape[1]
    for k in range(ncores):
        xs = np.ascontiguousarray(x[k * nb:(k + 1) * nb].reshape(nb * S, D))
        cs = np.ascontiguousarray(c[k * nb:(k + 1) * nb].reshape(nb, 8, 128).transpose(2, 1, 0))
        m = dict(shared)
        m["x"] = xs
        m["cT"] = cs
        maps.append(m)
    return maps


def kernel(x, c, w_ada, b_ada, w_in, conv_w, attn_norm_g, conv_norm_g, w_out, ln1_g, ln1_b,
           w_gate, w_up, w_down, ln2_g, ln2_b):
    ncores, nb = 8, 2
    maps = make_inputs(x, c, w_ada, b_ada, w_in, conv_w, attn_norm_g, conv_norm_g, w_out, ln1_g, ln1_b,
                       w_gate, w_up, w_down, ln2_g, ln2_b, nb, ncores)
    nc = build_program(nb, SEQ // 512)
    res = run_bass_kernel_spmd(nc, maps, core_ids=list(range(ncores)))
    outs = [np.asarray(r["out"], np.float32).reshape(nb, SEQ, D) for r in res.results]
    return np.concatenate(outs, axis=0)
```

```python
import math
import os
from contextlib import ExitStack
import numpy as np
import concourse.bass as bass
import concourse.mybir as mybir
from concourse.bass_utils import run_bass_kernel_spmd

F32 = mybir.dt.float32
BF16 = mybir.dt.bfloat16
AF = mybir.ActivationFunctionType
ALU = mybir.AluOpType
AX = mybir.AxisListType

D = 1024
SEQ = 4096
NHEAD = 8
DFF = 2816
NFF = DFF // 128
ALPHA = 2.0 ** 0.25
EPS = 1e-5
TOPK = 256
NBIS = 15
NEGBIG = -30000.0

OQ, OK_, OV, OQI, OKI, OWI, OGB, OGC, OHC = 0, 512, 576, 640, 896, 928, 936, 1448, 1960


def _sw(d):
    return d + 8 if d < 8 else d - 8


def build_chunks():
    chunks = {}
    order = []

    def add(name, cols):
        cols = list(cols) + [-1] * (128 - len(cols))
        chunks[name] = cols
        order.append(name)

    for i in range(4):
        A, B = 2 * i, 2 * i + 1
        cols = [OQ + A * 64 + d for d in range(16)] + [OQ + B * 64 + d for d in range(16)]
        cols += [OQ + A * 64 + d for d in range(16, 64)] + [OQ + B * 64 + d for d in range(16, 64)]
        add(f"q{i}", cols)
        sw = [OQ + A * 64 + _sw(d) for d in range(16)] + [OQ + B * 64 + _sw(d) for d in range(16)]
        add(f"qsw{i}", sw)
    kc = [OK_ + d for d in range(16)] * 2 + [OK_ + d for d in range(16, 64)] * 2
    add("kk", kc)
    add("kksw", [OK_ + _sw(d) for d in range(16)] * 2)
    for i in range(2):
        cols, sw = [], []
        for hp in range(4):
            h = 4 * i + hp
            cols += [OQI + h * 32 + d for d in range(32)]
            sw += [OQI + h * 32 + _sw(d) for d in range(16)] + [-1] * 16
        add(f"qi{i}", cols)
        add(f"qisw{i}", sw)
    add("ki3", [OKI + d for d in range(32)] * 3)
    add("ki3sw", ([OKI + _sw(d) for d in range(16)] + [-1] * 16) * 3)
    add("vwi", [OV + d for d in range(64)] + [OWI + d for d in range(8)])
    for j in range(4):
        add(f"gc{j}", [OGC + j * 128 + d for d in range(128)])
        add(f"hc{j}", [OHC + j * 128 + d for d in range(128)])
        add(f"gb{j}", [OGB + j * 128 + d for d in range(128)])
    return chunks, order


CHUNKS, CH_ORDER = build_chunks()
CH_IDX = {n: i for i, n in enumerate(CH_ORDER)}
NCH = len(CH_ORDER)

PV_INV, PV_NEG1, PV_NSGN, PV_NEGM, PV_ONEM, PV_NSGNM, PV_MA, PV_MB = range(8)
PV_CW = 8
PV_G = 20
PV_LN1G = 28
PV_LN1B = 36
PV_BADA = 44
PV_EPS = 76
PV_ZERO = 77
PV_P2 = 78
NPV = 102


class Tracker:
    def __init__(self, nc, nds=40):
        self.nc = nc
        self.eng = {"pe": nc.tensor, "act": nc.scalar, "dve": nc.vector, "pool": nc.gpsimd, "sp": nc.sync}
        self.sem = {k: nc.alloc_semaphore("s_" + k) for k in self.eng}
        self.cnt = {k: 0 for k in self.eng}
        self.known = {k: {} for k in self.eng}
        self.res = {}
        self.dsem = [nc.alloc_semaphore(f"d{i}") for i in range(nds)]
        self.dval = [0] * nds
        self.dnext = 0
        self.nds = nds

    def _semh(self, sk):
        return self.sem[sk[1]] if sk[0] == "e" else self.dsem[sk[1]]

    def _wait(self, en, sk, val):
        if val <= 0:
            return
        if self.known[en].get(sk, 0) >= val:
            return
        self.eng[en].wait_ge(self._semh(sk), val)
        self.known[en][sk] = val

    def _deps(self, en, reads, writes, is_dma):
        for k in reads:
            r = self.res.get(k)
            if r and r["w"]:
                sk, v = r["w"]
                if (not is_dma) and sk == ("e", en) and en == "pe":
                    continue
                self._wait(en, sk, v)
        for k in writes:
            r = self.res.get(k)
            if not r:
                continue
            if r["w"]:
                sk, v = r["w"]
                if not ((not is_dma) and sk == ("e", en) and en == "pe"):
                    self._wait(en, sk, v)
            for sk, v in r["r"].items():
                if (not is_dma) and sk == ("e", en) and en == "pe":
                    continue
                self._wait(en, sk, v)

    def _commit(self, tok, reads, writes):
        sk, v = tok
        for k in reads:
            r = self.res.setdefault(k, {"w": None, "r": {}})
            r["r"][sk] = max(r["r"].get(sk, 0), v)
        for k in writes:
            self.res[k] = {"w": tok, "r": {}}

    def op(self, en, fn, reads=(), writes=()):
        self._deps(en, reads, writes, False)
        ins = fn(self.eng[en])
        if isinstance(ins, (list, tuple)):
            ins = ins[-1]
        self.cnt[en] += 1
        ins.then_inc(self.sem[en], 1)
        self._commit((("e", en), self.cnt[en]), reads, writes)

    def dma(self, en, out, in_, reads=(), writes=()):
        if en == "pool":
            self.dsem.append(self.nc.alloc_semaphore(f"dq{len(self.dsem)}"))
            self.dval.append(0)
            s = len(self.dsem) - 1
        else:
            s = self.dnext
            self.dnext = (self.dnext + 1) % self.nds
        self._wait(en, ("d", s), self.dval[s])
        self._deps(en, reads, writes, True)
        self.dval[s] += 16
        self.eng[en].dma_start(out=out, in_=in_).then_inc(self.dsem[s], 16)
        self._commit((("d", s), self.dval[s]), reads, writes)

    def barrier(self):
        for en in self.eng:
            for e2 in self.eng:
                if e2 != en:
                    self._wait(en, ("e", e2), self.cnt[e2])
            for si in range(len(self.dsem)):
                self._wait(en, ("d", si), self.dval[si])

    def wait_all(self, en):
        for k, r in self.res.items():
            if r["w"]:
                self._wait(en, *r["w"])


class Pool:
    def __init__(self, name, tiles):
        self.name, self.tiles, self.i = name, tiles, 0

    def next(self):
        i = self.i
        self.i = (self.i + 1) % len(self.tiles)
        return self.tiles[i], (self.name, i)


class _Stop(Exception):
    pass


def build_program(nb, nblk, debug=False):
    try:
        return _build_program(nb, nblk, debug)
    except _Stop as st:
        T, nc = st.args
        for en in ("sp", "act", "pool"):
            T.wait_all(en)
        return nc


def _build_program(nb, nblk, debug=False):
    STOP = os.environ.get("KSTOP", "")
    S = nblk * 512
    NT = nb * S
    nc = bass.Bass("TRN2", target_bir_lowering=False)

    def din(name, shape, dt=F32):
        return nc.dram_tensor(name, list(shape), dt, kind="ExternalInput").ap()

    x_d = din("x", [NT, D])
    cT_d = din("cT", [128, 8, nb])
    wada_d = din("w_ada", [6, 128, 8 * 1024])
    badabc_d = din("b_ada_bc", [128, 2048])
    win_d = din("w_in", [NCH, 128, 8 * 128])
    wout_d = din("w_out", [128, 8, 1024])
    wgu_d = din("w_gu", [NFF, 128, 8 * 256])
    wdn_d = din("w_down", [128, NFF, 1024])
    pv_d = din("pvec", [128, NPV])
    lnbc_d = din("ln_bc", [128, 4, 1024])
    ident_d = din("ident", [128, 128])
    negi_d = din("negi4", [128, 512])
    sel_d = din("sel", [128, 2, 4, 96])
    bones_d = din("bones", [128, 128])
    nrm_d = din("nrm", [65, 64])
    caus_d = din("causneg", [128, 128])
    pos_d = din("posrow", [128, SEQ])
    out_d = nc.dram_tensor("out", [NT, D], F32, kind="ExternalOutput").ap()

    winbf = nc.dram_tensor("winbf", [NCH, 128, 8 * 128], BF16).ap()
    wgubf = nc.dram_tensor("wgubf", [NFF, 128, 8 * 256], BF16).ap()
    ropetab = nc.dram_tensor("ropetab", [128, 4, SEQ], F32).ap()
    gbc_d = nc.dram_tensor("gbc", [2 * nb, 128, 1024], F32).ap()
    y1_d = nc.dram_tensor("y1s", [NT, D], F32).ap()
    dbg = {}
    if debug:
        for nm, shp in (("d_h1T", [128, 8 * 512]), ("d_qpad", [128, 8 * 512]), ("d_KK", [128, 512]),
                        ("d_acc", [128, 512]), ("d_thr", [128, 8]), ("d_attnT", [128, 512]),
                        ("d_conv", [128, 4 * 512]), ("d_mv", [128, 64]), ("d_qi3", [128, 8 * 512]),
                        ("d_KI3", [128, 512]), ("d_rt", [128, 4 * 512]), ("d_z", [128, 1024])):
            dbg[nm] = nc.dram_tensor(nm, shp, F32, kind="ExternalOutput").ap()

    T = Tracker(nc)
    def sb(name, shape, dt):
        return nc.alloc_sbuf_tensor("sb_" + name, shape, dt)
    pv = sb("pv", [128, NPV], F32)
    ident = sb("ident", [128, 128], F32)
    negi = sb("negi", [128, 512], BF16)
    sel = sb("sel", [128, 2, 4, 96], BF16)
    bones = sb("bones", [128, 128], F32)
    nrm = sb("nrm", [65, 64], F32)
    caus = sb("caus", [128, 128], F32)
    mv = sb("mv", [128, 4, 8, nb], F32)
    s2 = sb("s2", [128, 2, 8, nb], F32)
    psum = [nc.alloc_psum_tensor(f"ps{i}", [128, 512], F32) for i in range(8)]
    PS = Pool("ps", psum[:6])
    psO = psum[6:8]
    tmps = [sb(f"tmp{i}", [128, 512], F32) for i in range(7)]
    TP = Pool("tmp", tmps)

    def load_const(dst, src, key, eng="sp"):
        T.dma(eng, dst, src, writes=[key])

    load_const(pv[:], pv_d, "pv")
    load_const(ident[:], ident_d, "ident")
    load_const(bones[:], bones_d, "bones")
    load_const(nrm[:], nrm_d, "nrm")
    load_const(caus[:], caus_d, "caus")
    load_const(negi[:], negi_d, "negi", "pool")
    load_const(sel[:], sel_d, "sel", "pool")
    for c0 in range(0, NCH, 8):
        c1 = min(NCH, c0 + 8)
        T.dma("pool", winbf[c0:c1], win_d[c0:c1], writes=["winbf"])
    for c0 in range(0, NFF, 6):
        c1 = min(NFF, c0 + 6)
        T.dma("pool", wgubf[c0:c1], wgu_d[c0:c1], writes=["wgubf"])

    with ExitStack() as es:
        wada_sb = es.enter_context(nc.sbuf_tensor("sb_wada_sb", [128, 8, 1024], BF16))
        cTs = es.enter_context(nc.sbuf_tensor("sb_cTs", [128, 8, nb], F32))
        cact = es.enter_context(nc.sbuf_tensor("sb_cact", [128, 8, nb], BF16))
        crep = es.enter_context(nc.sbuf_tensor("sb_crep", [128, 8, nb, 128], BF16))
        bbc = es.enter_context(nc.sbuf_tensor("sb_bbc", [128, 2048], F32))
        gtmp = es.enter_context(nc.sbuf_tensor("sb_gtmp", [128, 1024], F32))
        rtab = es.enter_context(nc.sbuf_tensor("sb_rtab", [128, 4, 512], F32))
        posb = es.enter_context(nc.sbuf_tensor("sb_posb", [128, 512], F32))
        T.dma("sp", cTs[:], cT_d, writes=["cTs"])
        T.dma("sp", bbc[:], badabc_d, writes=["bbc"])
        T.op("act", lambda e: e.activation(out=cact[:], in_=cTs[:], func=AF.Silu), ["cTs"], ["cact"])
        for kt in range(8):
            for b in range(nb):
                T.op("dve", lambda e: e.tensor_copy(out=crep[:, kt, b, :],
                                                    in_=cact[:, kt, b:b + 1].to_broadcast([128, 128])),
                     ["cact"], ["crep"])
        for j in range(6):
            T.dma("pool", wada_sb[:].rearrange("p k n -> p (k n)"), wada_d[j], writes=["wada"])
            if j in (2, 5):
                gi = 0 if j == 2 else 1
                for b in range(nb):
                    for half in range(2):
                        p, pk = PS.next()
                        T.op("pe", lambda e: [e.matmul(p[:, :], lhsT=crep[:, kt, b, :],
                                                       rhs=wada_sb[:, kt, half * 512:(half + 1) * 512],
                                                       start=(kt == 0), stop=(kt == 7)) for kt in range(8)],
                             ["crep", "wada"], [pk])
                        T.op("dve", lambda e: e.tensor_tensor(
                            out=gtmp[:, half * 512:(half + 1) * 512], in0=p[:, :],
                            in1=bbc[:, gi * 1024 + half * 512: gi * 1024 + (half + 1) * 512], op=ALU.add),
                             [pk, "bbc"], ["gtmp"])
                    T.dma("sp", gbc_d[gi * nb + b], gtmp[:], reads=["gtmp"], writes=["gbc"])
            else:
                idx = {0: 0, 1: 1, 3: 2, 4: 3}[j]
                for m in range(8):
                    p, pk = PS.next()
                    T.op("pe", lambda e: [e.matmul(p[:, 0:nb], lhsT=wada_sb[:, kt, m * 128:(m + 1) * 128],
                                                   rhs=cact[:, kt, :], start=(kt == 0), stop=(kt == 7))
                                          for kt in range(8)], ["cact", "wada"], [pk])
                    T.op("dve", lambda e: e.tensor_scalar(
                        out=mv[:, idx, m, :], in0=p[:, 0:nb], scalar1=pv[:, PV_BADA + idx * 8 + m: PV_BADA + idx * 8 + m + 1],
                        scalar2=(1.0 if idx in (1, 3) else 0.0), op0=ALU.add, op1=ALU.add), [pk, "pv"], ["mv"])
        for b in range(nb):
            T.op("dve", lambda e: e.tensor_tensor(out=s2[:, 0, :, b], in0=mv[:, 3, :, b],
                                                  in1=pv[:, PV_LN1G:PV_LN1G + 8], op=ALU.mult), ["mv", "pv"], ["s2a"])
            T.op("dve", lambda e: e.tensor_tensor(out=s2[:, 1, :, b], in0=mv[:, 3, :, b],
                                                  in1=pv[:, PV_LN1B:PV_LN1B + 8], op=ALU.mult), ["mv", "pv"], ["s2b"])
            T.op("dve", lambda e: e.tensor_tensor(out=s2[:, 1, :, b], in0=s2[:, 1, :, b],
                                                  in1=mv[:, 2, :, b], op=ALU.add), ["mv", "s2b"], ["s2b"])
        for c in range(nblk):
            T.dma("sp", posb[:], pos_d[:, c * 512:(c + 1) * 512], writes=["posb"])
            a0, a0k = TP.next()
            T.op("dve", lambda e: e.tensor_scalar(out=a0[:], in0=posb[:], scalar1=pv[:, PV_INV:PV_INV + 1],
                                                  scalar2=None, op0=ALU.mult), ["posb", "pv"], [a0k])
            a1, a1k = TP.next()
            a2, a2k = TP.next()
            C1 = 6.28125
            C2 = 2 * math.pi - C1
            MAGIC = 12582912.0
            for (aa, aak, shift) in ((a1, a1k, 0.0), (a2, a2k, 0.5 * math.pi)):
                kf, kfk = TP.next()
                T.op("dve", lambda e: e.tensor_scalar(out=aa[:], in0=a0[:], scalar1=shift, scalar2=None, op0=ALU.add),
                     [a0k], [aak])
                T.op("dve", lambda e: e.tensor_scalar(out=kf[:], in0=aa[:], scalar1=1.0 / (2 * math.pi), scalar2=None,
                                                      op0=ALU.mult), [aak], [kfk])
                T.op("dve", lambda e: e.tensor_scalar(out=kf[:], in0=kf[:], scalar1=MAGIC, scalar2=None, op0=ALU.add),
                     [kfk], [kfk])
                T.op("dve", lambda e: e.tensor_scalar(out=kf[:], in0=kf[:], scalar1=-MAGIC, scalar2=None, op0=ALU.add),
                     [kfk], [kfk])
                T.op("dve", lambda e: e.scalar_tensor_tensor(out=aa[:], in0=kf[:], scalar=-C1, in1=aa[:],
                                                             op0=ALU.mult, op1=ALU.add), [kfk, aak], [aak])
                T.op("dve", lambda e: e.scalar_tensor_tensor(out=aa[:], in0=kf[:], scalar=-C2, in1=aa[:],
                                                             op0=ALU.mult, op1=ALU.add), [kfk, aak], [aak])
                T.op("dve", lambda e: e.tensor_scalar(out=aa[:], in0=aa[:], scalar1=math.pi, scalar2=-math.pi,
                                                      op0=ALU.min, op1=ALU.max), [aak], [aak])
            T.op("act", lambda e: e.activation(out=a1[:], in_=a1[:], func=AF.Sin), [a1k], [a1k])
            T.op("act", lambda e: e.activation(out=a2[:], in_=a2[:], func=AF.Sin), [a2k], [a2k])

            def col(i):
                return pv[:, i:i + 1]
            T.op("dve", lambda e: e.tensor_scalar(out=rtab[:, 0, :], in0=a2[:], scalar1=col(PV_NEG1), scalar2=None,
                                                  op0=ALU.mult), [a2k, "pv"], ["rtab"])
            T.op("dve", lambda e: e.tensor_scalar(out=rtab[:, 1, :], in0=a1[:], scalar1=col(PV_NSGN), scalar2=None,
                                                  op0=ALU.mult), [a1k, "pv"], ["rtab"])
            T.op("dve", lambda e: e.tensor_scalar(out=rtab[:, 2, :], in0=a2[:], scalar1=col(PV_NEGM),
                                                  scalar2=col(PV_ONEM), op0=ALU.mult, op1=ALU.add), [a2k, "pv"], ["rtab"])
            T.op("dve", lambda e: e.tensor_scalar(out=rtab[:, 3, :], in0=a1[:], scalar1=col(PV_NSGNM), scalar2=None,
                                                  op0=ALU.mult), [a1k, "pv"], ["rtab"])
            T.dma("sp", ropetab[:, :, c * 512:(c + 1) * 512], rtab[:], reads=["rtab"], writes=["ropetab"])

    T.barrier()
    if STOP == "setup":
        raise _Stop(T, nc)
    with ExitStack() as es:
        wout_sb = es.enter_context(nc.sbuf_tensor("sb_wout_sb", [128, 8, 1024], BF16))
        wring = es.enter_context(nc.sbuf_tensor("sb_wring", [128, 4, 8, 128], BF16))
        KK = es.enter_context(nc.sbuf_tensor("sb_KK", [128, SEQ], BF16))
        KI3 = es.enter_context(nc.sbuf_tensor("sb_KI3", [96, SEQ], BF16))
        Vaug = es.enter_context(nc.sbuf_tensor("sb_Vaug", [128, 32, 65], BF16))
        wi_sb = es.enter_context(nc.sbuf_tensor("sb_wi_sb", [128, 32, 8], F32))
        rt = es.enter_context(nc.sbuf_tensor("sb_rt", [128, 4, 512], F32))
        xb = es.enter_context(nc.sbuf_tensor("sb_xb", [128, 4, 1024], F32))
        h1T = es.enter_context(nc.sbuf_tensor("sb_h1T", [128, 8, 512], BF16))
        qpad = es.enter_context(nc.sbuf_tensor("sb_qpad", [128, 2, 2, 4, 512], BF16))
        qi3 = es.enter_context(nc.sbuf_tensor("sb_qi3", [96, 8, 512], BF16))
        convb = es.enter_context(nc.sbuf_tensor("sb_convb", [128, 2, 4, 512], BF16))
        hi_t = es.enter_context(nc.sbuf_tensor("sb_hi_t", [128, 2, 512], BF16))
        lo_t = es.enter_context(nc.sbuf_tensor("sb_lo_t", [128, 2, 512], BF16))
        klo = es.enter_context(nc.sbuf_tensor("sb_klo", [128, 512], BF16))
        ubuf = es.enter_context(nc.sbuf_tensor("sb_ubuf", [128, 514], F32))
        halo = es.enter_context(nc.sbuf_tensor("sb_halo", [128, 4, 2], F32))
        xr = es.enter_context(nc.sbuf_tensor("sb_xr", [128, 1024], F32))
        acc = es.enter_context(nc.sbuf_tensor("sb_acc", [128, SEQ], F32))
        Rc = es.enter_context(nc.sbuf_tensor("sb_Rc", [128, 2, 1024], F32))
        nmask = es.enter_context(nc.sbuf_tensor("sb_nmask", [128, SEQ], BF16))
        nmaskb = es.enter_context(nc.sbuf_tensor("sb_nmaskb", [128, SEQ], BF16))
        rngk = es.enter_context(nc.sbuf_tensor("sb_rngk", [128, 32], F32))
        PT = es.enter_context(nc.sbuf_tensor("sb_PT", [128, 2, 1024], BF16))
        attnT = es.enter_context(nc.sbuf_tensor("sb_attnT", [128, 2, 4, 128], BF16))
        zt = es.enter_context(nc.sbuf_tensor("sb_zt", [128, 1024], F32))
        yh = es.enter_context(nc.sbuf_tensor("sb_yh", [128, 2, 1024], F32))
        g1bc = es.enter_context(nc.sbuf_tensor("sb_g1bc", [128, 1024], F32))
        bis = es.enter_context(nc.sbuf_tensor("sb_bis", [128, 16], F32))
        stat = es.enter_context(nc.sbuf_tensor("sb_stat", [128, 16], F32))
        for kt in range(8):
            wstage, wsk = TP.next()
            for hf in range(2):
                T.dma("sp", wstage[:], wout_d[:, kt, hf * 512:(hf + 1) * 512], writes=[wsk])
                T.op("dve", lambda e: e.tensor_scalar(out=wout_sb[:, kt, hf * 512:(hf + 1) * 512], in0=wstage[:],
                                                      scalar1=pv[:, PV_G + kt:PV_G + kt + 1], scalar2=None, op0=ALU.mult),
                     [wsk, "pv"], ["wout"])
        T.op("pool", lambda e: e.memset(Vaug[:], 1.0), [], ["Vaug"])
        T.op("dve", lambda e: e.memset(bis[:], 0.0), [], ["thr", "bmn", "bmx", "brng", "cand", "cnt", "bt"])

        ring_i = [0]

        def wload(name):
            ci = CH_IDX[name]
            s = ring_i[0]
            ring_i[0] = (s + 1) % 4
            key = ("wring", s)
            T.dma("sp", wring[:, s, :, :].rearrange("p k n -> p (k n)"), winbf[ci], reads=["winbf"], writes=[key])
            return wring[:, s, :, :], key

        def proj(name, M):
            w, wk = wload(name)
            p, pk = PS.next()
            T.op("pe", lambda e: [e.matmul(p[0:M, :], lhsT=w[:, kt, 0:M], rhs=h1T[:, kt, :],
                                           start=(kt == 0), stop=(kt == 7)) for kt in range(8)],
                 [wk, "h1T"], [pk])
            return p, pk

        def rope(p, pk, psw, pswk, rows, ci, si):
            t1, t1k = TP.next()
            t2, t2k = TP.next()
            T.op("act", lambda e: e.activation(out=t1[0:rows, :], in_=p[0:rows, :], func=AF.Copy), [pk], [t1k])
            T.op("act", lambda e: e.activation(out=t2[0:rows, :], in_=psw[0:rows, :], func=AF.Copy), [pswk], [t2k])
            T.op("dve", lambda e: e.tensor_tensor(out=t1[0:rows, :], in0=t1[0:rows, :], in1=rt[0:rows, ci, :],
                                                  op=ALU.mult), [t1k, "rt"], [t1k])
            T.op("dve", lambda e: e.tensor_tensor(out=t2[0:rows, :], in0=t2[0:rows, :], in1=rt[0:rows, si, :],
                                                  op=ALU.mult), [t2k, "rt"], [t2k])
            T.op("dve", lambda e: e.tensor_tensor(out=t1[0:rows, :], in0=t1[0:rows, :], in1=t2[0:rows, :],
                                                  op=ALU.add), [t1k, t2k], [t1k])
            return t1, t1k

        def phaseA(b, Bb, part):
            bp = Bb % 2
            T0 = b * S + Bb * 512
            P0 = Bb * 512
            if part == 1:
                T.dma("sp", rt[:], ropetab[:, :, P0:P0 + 512], reads=["ropetab"], writes=["rt"])
                for tt in range(4):
                    T.dma("sp", xb[:, tt, :], x_d[T0 + tt * 128:T0 + (tt + 1) * 128, :], writes=[("xb", tt)])
                xkeys = [("xb", tt) for tt in range(4)]
                for kt in range(8):
                    p, pk = PS.next()
                    T.op("pe", lambda e: [e.transpose(out=p[:, tt * 128:(tt + 1) * 128],
                                                      in_=xb[:, tt, kt * 128:(kt + 1) * 128], identity=ident[:])
                                          for tt in range(4)], xkeys + ["ident"], [pk])
                    T.op("act", lambda e: e.activation(out=h1T[:, kt, :], in_=p[:, :], func=AF.Identity,
                                                       scale=mv[:, 1, kt, b:b + 1], bias=mv[:, 0, kt, b:b + 1]),
                         [pk, "mv"], ["h1T"])
                p, pk = proj("kk", 128)
                psw, pswk = proj("kksw", 32)
                t1, t1k = rope(p, pk, psw, pswk, 32, 0, 1)
                T.op("act", lambda e: e.activation(out=KK[:, P0:P0 + 512], in_=p[:, :], func=AF.Copy), [pk], ["KK"])
                T.op("dve", lambda e: e.tensor_copy(out=KK[0:32, P0:P0 + 512], in_=t1[0:32, :]), [t1k], ["KK"])
                w, wk = wload("vwi")
                for tt in range(4):
                    p, pk = PS.next()
                    T.op("pe", lambda e: [e.matmul(p[:, 0:64], lhsT=h1T[:, kt, tt * 128:(tt + 1) * 128], rhs=w[:, kt, 0:64],
                                                   start=(kt == 0), stop=(kt == 7)) for kt in range(8)] +
                                         [e.matmul(p[:, 64:72], lhsT=h1T[:, kt, tt * 128:(tt + 1) * 128], rhs=w[:, kt, 64:72],
                                                   start=(kt == 0), stop=(kt == 7)) for kt in range(8)],
                         [wk, "h1T"], [pk])
                    kb = Bb * 4 + tt
                    T.op("act", lambda e: e.activation(out=Vaug[:, kb, 0:64], in_=p[:, 0:64], func=AF.Copy), [pk], ["Vaug"])
                    T.op("dve", lambda e: e.tensor_scalar(out=wi_sb[:, kb, :], in0=p[:, 64:72], scalar1=0.0625, scalar2=None,
                                                          op0=ALU.mult), [pk, "Vaug"], ["wi"])
                for j in range(4):
                    uk = "u"
                    hk_ = ("halo", j)
                    if Bb == 0:
                        T.op("pool", lambda e: e.memset(ubuf[:, 0:2], 0.0), [], [uk])
                    else:
                        T.op("pool", lambda e: e.tensor_copy(out=ubuf[:, 0:2], in_=halo[:, j, :]), [hk_], [uk])
                    pgc, pgck = proj(f"gc{j}", 128)
                    phc, phck = proj(f"hc{j}", 128)
                    pgb, pgbk = proj(f"gb{j}", 128)
                    g, gk = TP.next()
                    T.op("act", lambda e: e.activation(out=g[:], in_=pgc[:, :], func=AF.Copy), [pgck], [gk])
                    T.op("dve", lambda e: e.tensor_tensor(out=ubuf[:, 2:514], in0=phc[:, :], in1=g[:], op=ALU.mult),
                         [phck, gk], [uk])
                    c0, c0k = TP.next()
                    cw = PV_CW + j * 3
                    T.op("act", lambda e: e.activation(out=c0[:], in_=ubuf[:, 2:514], func=AF.Identity,
                                                       scale=pv[:, cw + 2:cw + 3], bias=pv[:, PV_ZERO:PV_ZERO + 1]), [uk, "pv"], [c0k])
                    T.op("dve", lambda e: e.scalar_tensor_tensor(out=c0[:], in0=ubuf[:, 1:513], scalar=pv[:, cw + 1:cw + 2],
                                                                 in1=c0[:], op0=ALU.mult, op1=ALU.add), [uk, c0k, "pv"], [c0k])
                    T.op("dve", lambda e: e.scalar_tensor_tensor(out=c0[:], in0=ubuf[:, 0:512], scalar=pv[:, cw:cw + 1],
                                                                 in1=c0[:], op0=ALU.mult, op1=ALU.add), [uk, c0k, "pv"], [c0k])
                    T.op("dve", lambda e: e.tensor_tensor(out=c0[:], in0=pgb[:, :], in1=c0[:], op=ALU.mult), [pgbk, c0k], [c0k])
                    ysq, ysqk = TP.next()
                    T.op("act", lambda e: e.activation(out=ysq[:], in_=c0[:], func=AF.Square), [c0k], [ysqk])
                    pss, pssk = PS.next()
                    T.op("pe", lambda e: e.matmul(pss[:, :], lhsT=bones[:], rhs=ysq[:], start=True, stop=True),
                         ["bones", ysqk], [pssk])
                    r, rk = TP.next()
                    T.op("act", lambda e: e.activation(out=r[:], in_=pss[:, :], func=AF.Ln, bias=pv[:, PV_EPS:PV_EPS + 1]),
                         [pssk, "pv"], [rk])
                    T.op("act", lambda e: e.activation(out=r[:], in_=r[:], func=AF.Exp, scale=-0.5), [rk], [rk])
                    T.op("dve", lambda e: e.tensor_tensor(out=convb[:, bp, j, :], in0=c0[:], in1=r[:], op=ALU.mult),
                         [c0k, rk], [("convb", bp)])
                    T.op("pool", lambda e: e.tensor_copy(out=halo[:, j, :], in_=ubuf[:, 512:514]), [uk], [hk_])

            if part == 2:
                for i in range(4):
                    p, pk = proj(f"q{i}", 128)
                    psw, pswk = proj(f"qsw{i}", 32)
                    t1, t1k = rope(p, pk, psw, pswk, 32, 0, 1)
                    T.op("act", lambda e: [e.activation(out=qpad[:, bp, 0, i, :], in_=p[:, :], func=AF.Identity,
                                                        scale=pv[:, PV_MA:PV_MA + 1], bias=pv[:, PV_ZERO:PV_ZERO + 1]),
                                           e.activation(out=qpad[:, bp, 1, i, :], in_=p[:, :], func=AF.Identity,
                                                        scale=pv[:, PV_MB:PV_MB + 1], bias=pv[:, PV_ZERO:PV_ZERO + 1])], [pk, "pv"], [("qpad", bp)])
                    T.op("dve", lambda e: [e.tensor_scalar(out=qpad[0:32, bp, 0, i, :], in0=t1[0:32, :],
                                                           scalar1=pv[0:32, PV_MA:PV_MA + 1], scalar2=None, op0=ALU.mult),
                                           e.tensor_scalar(out=qpad[0:32, bp, 1, i, :], in0=t1[0:32, :],
                                                           scalar1=pv[0:32, PV_MB:PV_MB + 1], scalar2=None, op0=ALU.mult)],
                         [t1k, "pv"], [("qpad", bp)])
                for i in range(2):
                    p, pk = proj(f"qi{i}", 128)
                    psw, pswk = proj(f"qisw{i}", 128)
                    t1, t1k = rope(p, pk, psw, pswk, 128, 2, 3)
                    T.op("act", lambda e: e.activation(out=hi_t[:, i, :], in_=t1[:, :], func=AF.Copy), [t1k], [("hi", i)])
                    T.op("dve", lambda e: e.tensor_tensor(out=lo_t[:, i, :], in0=t1[:, :], in1=hi_t[:, i, :],
                                                          op=ALU.subtract), [t1k, ("hi", i)], [("lo", i)])
                for h in range(8):
                    i, hp = h // 4, h % 4
                    p, pk = PS.next()
                    T.op("pe", lambda e: [e.matmul(p[0:96, :], lhsT=sel[:, 0, hp, :], rhs=hi_t[:, i, :], start=True, stop=False),
                                          e.matmul(p[0:96, :], lhsT=sel[:, 1, hp, :], rhs=lo_t[:, i, :], start=False, stop=True)],
                         ["sel", ("hi", i), ("lo", i)], [pk])
                    T.op("act", lambda e: e.activation(out=qi3[0:96, h, :], in_=p[0:96, :], func=AF.Copy), [pk], ["qi3"])
                p, pk = proj("ki3", 96)
                psw, pswk = proj("ki3sw", 96)
                t1, t1k = rope(p, pk, psw, pswk, 96, 2, 3)
                T.op("act", lambda e: e.activation(out=KI3[0:96, P0:P0 + 512], in_=t1[0:96, :], func=AF.Copy), [t1k], ["KI3"])
                T.op("dve", lambda e: e.tensor_tensor(out=klo[64:96, :], in0=t1[64:96, :], in1=KI3[64:96, P0:P0 + 512],
                                                      op=ALU.subtract), [t1k, "KI3"], ["klo"])
                T.op("dve", lambda e: e.tensor_copy(out=KI3[64:96, P0:P0 + 512], in_=klo[64:96, :]), ["klo"], ["KI3"])

        PS_idx = Pool("ps", psum[0:2])
        PS_att = Pool("ps", psum[2:4])
        PS_att.off = 2
        PS_misc = Pool("ps", psum[4:6])
        PS_misc.off = 4
        nmask2 = [nmask, nmaskb]

        def phF(b, Bb, qq):
            qb = Bb * 4 + qq
            Sk = (qb + 1) * 128
            qs = slice(qq * 128, (qq + 1) * 128)
            nm = nmask2[qb % 2]
            nmk = ("nmask", qb % 2)
            nch = (Sk + 1023) // 1024
            for h in range(8):
                for c in range(nch):
                    c0_ = c * 1024
                    wd = min(1024, Sk - c0_)
                    rcs = (h * nch + c) % 2
                    rk = ("Rc", rcs)
                    for sub in range((wd + 511) // 512):
                        s0 = c0_ + sub * 512
                        w2 = min(512, Sk - s0)
                        p, pk = PS_idx.next()
                        T.op("pe", lambda e: e.matmul(p[:, 0:w2], lhsT=qi3[0:96, h, qs], rhs=KI3[0:96, s0:s0 + w2],
                                                      start=True, stop=True), ["qi3", "KI3"], [pk])
                        T.op("act", lambda e: e.activation(out=Rc[:, rcs, sub * 512:sub * 512 + w2], in_=p[:, 0:w2],
                                                           func=AF.Relu), [pk], [rk])
                    if h == 0:
                        T.op("dve", lambda e: e.tensor_scalar(out=acc[:, c0_:c0_ + wd], in0=Rc[:, rcs, 0:wd],
                                                              scalar1=wi_sb[:, qb, 0:1], scalar2=None, op0=ALU.mult),
                             [rk, "wi"], ["acc"])
                    else:
                        T.op("dve", lambda e: e.scalar_tensor_tensor(out=acc[:, c0_:c0_ + wd], in0=Rc[:, rcs, 0:wd],
                                                                     scalar=wi_sb[:, qb, h:h + 1], in1=acc[:, c0_:c0_ + wd],
                                                                     op0=ALU.mult, op1=ALU.add), [rk, "wi", "acc"], ["acc"])
            thr = bis[:, 0:1]
            if Sk <= TOPK:
                T.op("dve", lambda e: e.tensor_tensor(out=acc[:, Sk - 128:Sk], in0=acc[:, Sk - 128:Sk], in1=caus[:],
                                                      op=ALU.add), ["acc", "caus"], ["acc"])
                T.op("dve", lambda e: e.memset(thr, -1.0e29), [], ["thr"])
            else:
                T.op("dve", lambda e: [e.tensor_reduce(out=bis[:, 1:2], in_=acc[:, 0:Sk], axis=AX.X, op=ALU.min),
                                       e.tensor_reduce(out=bis[:, 2:3], in_=acc[:, 0:Sk], axis=AX.X, op=ALU.max)],
                     ["acc"], ["bmnx"])
                T.op("dve", lambda e: e.tensor_tensor(out=acc[:, Sk - 128:Sk], in0=acc[:, Sk - 128:Sk], in1=caus[:],
                                                      op=ALU.add), ["acc", "caus", "bmnx"], ["acc"])
                T.op("dve", lambda e: e.tensor_tensor(out=bis[:, 3:4], in0=bis[:, 2:3], in1=bis[:, 1:2], op=ALU.subtract),
                     ["bmnx"], ["brng"])
                T.op("dve", lambda e: e.tensor_scalar(out=bis[:, 3:4], in0=bis[:, 3:4], scalar1=1.001, scalar2=1e-6,
                                                      op0=ALU.mult, op1=ALU.add), ["brng"], ["brng"])
                T.op("dve", lambda e: [e.tensor_scalar(out=rngk[:, 0:NBIS + 2], in0=pv[:, PV_P2:PV_P2 + NBIS + 2],
                                                       scalar1=bis[:, 3:4], scalar2=None, op0=ALU.mult),
                                       e.scalar_tensor_tensor(out=bis[:, 4:5], in0=bis[:, 3:4], scalar=0.5, in1=bis[:, 1:2],
                                                              op0=ALU.mult, op1=ALU.add)],
                     ["brng", "bmnx", "pv"], ["rngk", "cand"])
                for it in range(1, NBIS + 1):
                    T.op("dve", lambda e: e.tensor_scalar(out=nm[:, 0:Sk], in0=acc[:, 0:Sk], scalar1=bis[:, 4:5],
                                                          scalar2=None, op0=ALU.is_ge, op1=ALU.add, accum_out=bis[:, 5:6]),
                         ["acc", "cand"], [nmk, "cnt"])
                    T.op("dve", lambda e: e.tensor_scalar(out=bis[:, 6:7], in0=bis[:, 5:6], scalar1=TOPK - 0.5, scalar2=0.5,
                                                          op0=ALU.is_ge, op1=ALU.subtract), ["cnt"], ["bt"])
                    T.op("dve", lambda e: e.scalar_tensor_tensor(out=bis[:, 4:5], in0=bis[:, 6:7], scalar=rngk[:, it:it + 1],
                                                                 in1=bis[:, 4:5], op0=ALU.mult, op1=ALU.add),
                         ["bt", "rngk", "cand"], ["cand"])
                T.op("dve", lambda e: e.tensor_tensor(out=thr, in0=bis[:, 4:5], in1=rngk[:, NBIS + 1:NBIS + 2], op=ALU.subtract),
                     ["cand", "rngk"], ["thr"])
            T.op("dve", lambda e: e.tensor_scalar(out=nm[:, 0:Sk], in0=acc[:, 0:Sk], scalar1=thr, scalar2=None,
                                                  op0=ALU.is_lt), ["acc", "thr"], [nmk])

        def phB1(b, Bb, qq):
            qb = Bb * 4 + qq
            bp = Bb % 2
            qs = slice(qq * 128, (qq + 1) * 128)
            nm = nmask2[qb % 2]
            nmk = ("nmask", qb % 2)
            for kb in range(qb + 1):
                ks = slice(kb * 128, (kb + 1) * 128)
                pts = kb % 2
                ptk = ("PT", pts)
                for par in range(2):
                    p, pk = PS_att.next()
                    pk = ("ps", pk[1] + 2)
                    T.op("pe", lambda e: [e.matmul(p[:, :].rearrange("p (a t) -> p a t", a=4), lhsT=KK[:, ks],
                                                   rhs=qpad[:, bp, par, :, qs], start=True, stop=False),
                                          e.matmul(p[:, :], lhsT=nm[:, ks], rhs=negi[:], start=False, stop=True)],
                         ["KK", ("qpad", bp), nmk, "negi"], [pk])
                    T.op("act", lambda e: e.activation(out=PT[:, pts, par * 512:(par + 1) * 512], in_=p[:, :], func=AF.Exp,
                                                       scale=0.125), [pk], [ptk])
                T.op("pe", lambda e: [e.matmul(psO[par][0:65, :], lhsT=Vaug[:, kb, :], rhs=PT[:, pts, par * 512:(par + 1) * 512],
                                               start=(kb == 0), stop=(kb == qb)) for par in range(2)],
                     [ptk, "Vaug"], ["psO"])

        def phB1b(b, Bb, qq):
            qb = Bb * 4 + qq
            rr = []
            for par in range(2):
                sq, sqk = TP.next()
                T.op("act", lambda e: e.activation(out=sq[0:65, :], in_=psO[par][0:65, :], func=AF.Square), ["psO"], [sqk])
                pn, pnk = PS_misc.next()
                pnk = ("ps", pnk[1] + 4)
                T.op("pe", lambda e: e.matmul(pn[0:64, :], lhsT=nrm[:, :], rhs=sq[0:65, :], start=True, stop=True),
                     ["nrm", sqk], [pnk])
                r, rk2 = TP.next()
                T.op("act", lambda e: e.activation(out=r[0:64, :], in_=pn[0:64, :], func=AF.Ln), [pnk], [rk2])
                T.op("act", lambda e: e.activation(out=r[0:64, :], in_=r[0:64, :], func=AF.Exp, scale=-0.5), [rk2], [rk2])
                rr.append((r, rk2))
            for par in range(2):
                r, rk2 = rr[par]
                T.op("dve", lambda e: e.tensor_tensor(out=attnT[par * 64:(par + 1) * 64, qb % 2, :, :].rearrange("p a t -> p (a t)"),
                                                      in0=psO[par][0:64, :], in1=r[0:64, :], op=ALU.mult),
                     ["psO", rk2], [("attnT", qb % 2)])

        def phB2(b, Bb, qq):
            qb = Bb * 4 + qq
            bp = Bb % 2
            T0 = b * S + qb * 128
            qs = slice(qq * 128, (qq + 1) * 128)
            T.dma("sp", xr[:], x_d[T0:T0 + 128, :], writes=["xr"])
            for half in range(2):
                hs = slice(half * 512, (half + 1) * 512)
                p, pk = PS_misc.next()
                pk = ("ps", pk[1] + 4)
                T.op("pe", lambda e: [e.matmul(p[:, :], lhsT=(attnT[:, qb % 2, kt, :] if kt < 4 else convb[:, bp, kt - 4, qs]),
                                               rhs=wout_sb[:, kt, hs], start=(kt == 0), stop=(kt == 7)) for kt in range(8)],
                     [("attnT", qb % 2), ("convb", bp), "wout"], [pk])
                T.op("dve", lambda e: e.tensor_tensor(out=zt[:, hs], in0=p[:, :], in1=g1bc[:, hs], op=ALU.mult),
                     [pk, "g1bc"], ["zt"])
            T.op("dve", lambda e: e.scalar_tensor_tensor(out=zt[:], in0=xr[:], scalar=ALPHA, in1=zt[:],
                                                         op0=ALU.mult, op1=ALU.add), ["xr", "zt"], ["zt"])
            ys = qb % 2
            layer_norm(zt, "zt", yh[:, ys, :], ("yh", ys))
            T.dma("sp", y1_d[T0:T0 + 128, :], yh[:, ys, :], reads=[("yh", ys)], writes=[("y1s", b, qb)])

        def layer_norm(src, srck, dst, dstk):
            T.op("dve", lambda e: [e.bn_stats(out=stat[:, 0:6], in_=src[:, 0:512]),
                                   e.bn_stats(out=stat[:, 6:12], in_=src[:, 512:1024])], [srck], ["bnst"])
            T.op("dve", lambda e: e.bn_aggr(out=stat[:, 12:14], in_=stat[:, 0:12]), ["bnst"], ["bnag"])
            T.op("act", lambda e: e.activation(out=stat[:, 14:15], in_=stat[:, 13:14], func=AF.Ln,
                                               bias=pv[:, PV_EPS:PV_EPS + 1]), ["bnag", "pv"], ["rstd"])
            T.op("act", lambda e: e.activation(out=stat[:, 14:15], in_=stat[:, 14:15], func=AF.Exp, scale=-0.5),
                 ["rstd"], ["rstd"])
            T.op("dve", lambda e: e.tensor_scalar(out=dst, in0=src[:], scalar1=stat[:, 12:13], scalar2=stat[:, 14:15],
                                                  op0=ALU.subtract, op1=ALU.mult), [srck, "bnag", "rstd"], [dstk])

        for b in range(nb):
            T.dma("sp", g1bc[:], gbc_d[0 * nb + b], reads=["gbc"], writes=["g1bc"])
            NQ = nblk * 4
            phaseA(b, 0, 1)
            phaseA(b, 0, 2)
            for q in range(NQ + 2):
                if q < NQ:
                    phF(b, q // 4, q % 4)
                if 0 <= q - 1 < NQ:
                    phB1(b, (q - 1) // 4, (q - 1) % 4)
                if q < NQ:
                    if q % 4 == 2 and q + 2 < NQ:
                        phaseA(b, (q + 2) // 4, 1)
                    if q % 4 == 3 and q + 1 < NQ:
                        phaseA(b, (q + 1) // 4, 2)
                if 0 <= q - 1 < NQ:
                    phB1b(b, (q - 1) // 4, (q - 1) % 4)
                if 0 <= q - 2 < NQ:
                    phB2(b, (q - 2) // 4, (q - 2) % 4)

    T.barrier()
    if STOP == "s1":
        raise _Stop(T, nc)
    with ExitStack() as es:
        wdn_sb = es.enter_context(nc.sbuf_tensor("sb_wdn_sb", [128, NFF, 1024], BF16))
        gring = es.enter_context(nc.sbuf_tensor("sb_gring", [128, 4, 8, 256], BF16))
        yb = es.enter_context(nc.sbuf_tensor("sb_yb", [128, 2, 4, 1024], F32))
        h2T = es.enter_context(nc.sbuf_tensor("sb_h2T", [128, 2, 8, 512], BF16))
        actT = es.enter_context(nc.sbuf_tensor("sb_actT", [128, NFF, 512], BF16))
        lnbc = es.enter_context(nc.sbuf_tensor("sb_lnbc", [128, 4, 1024], F32))
        g2bc = es.enter_context(nc.sbuf_tensor("sb_g2bc", [128, 2, 1024], F32))
        z2 = es.enter_context(nc.sbuf_tensor("sb_z2", [128, 1024], F32))
        y1t = es.enter_context(nc.sbuf_tensor("sb_y1t", [128, 1024], F32))
        ot = es.enter_context(nc.sbuf_tensor("sb_ot", [128, 2, 1024], F32))
        stat = es.enter_context(nc.sbuf_tensor("sb_stat2", [128, 16], F32))
        T.dma("pool", wdn_sb[:], wdn_d, writes=["wdn"])
        T.dma("sp", lnbc[:], lnbc_d, writes=["lnbc"])
        gi = [0]

        def layer_norm2(src, srck, dst, dstk):
            T.op("dve", lambda e: [e.bn_stats(out=stat[:, 0:6], in_=src[:, 0:512]),
                                   e.bn_stats(out=stat[:, 6:12], in_=src[:, 512:1024])], [srck], ["bnst"])
            T.op("dve", lambda e: e.bn_aggr(out=stat[:, 12:14], in_=stat[:, 0:12]), ["bnst"], ["bnag"])
            T.op("act", lambda e: e.activation(out=stat[:, 14:15], in_=stat[:, 13:14], func=AF.Ln,
                                               bias=pv[:, PV_EPS:PV_EPS + 1]), ["bnag", "pv"], ["rstd"])
            T.op("act", lambda e: e.activation(out=stat[:, 14:15], in_=stat[:, 14:15], func=AF.Exp, scale=-0.5),
                 ["rstd"], ["rstd"])
            T.op("dve", lambda e: e.tensor_scalar(out=dst, in0=src[:], scalar1=stat[:, 12:13], scalar2=stat[:, 14:15],
                                                  op0=ALU.subtract, op1=ALU.mult), [srck, "bnag", "rstd"], [dstk])

        blocks = [(b, Bb) for b in range(nb) for Bb in range(nblk)]
        for b in range(nb):
            T.dma("sp", g2bc[:, b, :], gbc_d[1 * nb + b], reads=["gbc"], writes=[("g2bc", b)])

        def load_y(i):
            b, Bb = blocks[i]
            T0 = b * S + Bb * 512
            for tt in range(4):
                T.dma("sp", yb[:, i % 2, tt, :], y1_d[T0 + tt * 128:T0 + (tt + 1) * 128, :],
                      reads=[("y1s", b, Bb * 4 + tt)], writes=[("yb", i % 2, tt)])

        def transp(i):
            b, Bb = blocks[i]
            ykeys = [("yb", i % 2, tt) for tt in range(4)]
            for kt in range(8):
                p, pk = PS.next()
                T.op("pe", lambda e: [e.transpose(out=p[:, tt * 128:(tt + 1) * 128],
                                                  in_=yb[:, i % 2, tt, kt * 128:(kt + 1) * 128], identity=ident[:])
                                      for tt in range(4)], ykeys + ["ident"], [pk])
                T.op("act", lambda e: e.activation(out=h2T[:, i % 2, kt, :], in_=p[:, :], func=AF.Identity,
                                                   scale=s2[:, 0, kt, b:b + 1], bias=s2[:, 1, kt, b:b + 1]),
                     [pk, "s2a", "s2b"], [("h2T", i % 2)])

        load_y(0)
        transp(0)
        if len(blocks) > 1:
            load_y(1)
        for i, (b, Bb) in enumerate(blocks):
            T0 = b * S + Bb * 512
            hk = ("h2T", i % 2)
            for m in range(NFF):
                if m == 8 and i >= 1 and i + 1 < len(blocks):
                    load_y(i + 1)
                s_ = gi[0]
                gi[0] = (s_ + 1) % 4
                gk = ("gring", s_)
                T.dma("sp", gring[:, s_, :, :].rearrange("p k n -> p (k n)"), wgubf[m], reads=["wgubf"], writes=[gk])
                pg, pgk = PS.next()
                pu, puk = PS.next()
                T.op("pe", lambda e: [e.matmul(pg[:, :], lhsT=gring[:, s_, kt, 0:128], rhs=h2T[:, i % 2, kt, :],
                                               start=(kt == 0), stop=(kt == 7)) for kt in range(8)] +
                                     [e.matmul(pu[:, :], lhsT=gring[:, s_, kt, 128:256], rhs=h2T[:, i % 2, kt, :],
                                               start=(kt == 0), stop=(kt == 7)) for kt in range(8)],
                     [gk, hk], [pgk, puk])
                sg, sgk = TP.next()
                T.op("act", lambda e: e.activation(out=sg[:], in_=pg[:, :], func=AF.Silu), [pgk], [sgk])
                T.op("dve", lambda e: e.tensor_tensor(out=actT[:, m, :], in0=pu[:, :], in1=sg[:], op=ALU.mult),
                     [puk, sgk], ["actT"])
            if i + 1 < len(blocks):
                transp(i + 1)
            for tt in range(4):
                ts_ = slice(tt * 128, (tt + 1) * 128)
                for half in range(2):
                    hs = slice(half * 512, (half + 1) * 512)
                    p, pk = PS.next()
                    T.op("pe", lambda e: [e.matmul(p[:, :], lhsT=actT[:, m, ts_], rhs=wdn_sb[:, m, hs],
                                                   start=(m == 0), stop=(m == NFF - 1)) for m in range(NFF)],
                         ["actT", "wdn"], [pk])
                    T.op("dve", lambda e: e.tensor_tensor(out=z2[:, hs], in0=p[:, :], in1=g2bc[:, b, hs], op=ALU.mult),
                         [pk, ("g2bc", b)], ["z2"])
                T.op("pool", lambda e: e.tensor_tensor(out=y1t[:], in0=yb[:, i % 2, tt, :], in1=lnbc[:, 0, :], op=ALU.mult),
                     [("yb", i % 2, tt), "lnbc"], ["y1t"])
                T.op("pool", lambda e: e.tensor_tensor(out=y1t[:], in0=y1t[:], in1=lnbc[:, 1, :], op=ALU.add),
                     ["y1t", "lnbc"], ["y1t"])
                T.op("dve", lambda e: e.scalar_tensor_tensor(out=z2[:], in0=y1t[:], scalar=ALPHA, in1=z2[:],
                                                             op0=ALU.mult, op1=ALU.add), ["y1t", "z2"], ["z2"])
                os_ = tt % 2
                ok = ("ot", os_)
                layer_norm2(z2, "z2", ot[:, os_, :], ok)
                T.op("pool", lambda e: e.tensor_tensor(out=ot[:, os_, :], in0=ot[:, os_, :], in1=lnbc[:, 2, :], op=ALU.mult),
                     [ok, "lnbc"], [ok])
                T.op("pool", lambda e: e.tensor_tensor(out=ot[:, os_, :], in0=ot[:, os_, :], in1=lnbc[:, 3, :], op=ALU.add),
                     [ok, "lnbc"], [ok])
                T.dma("sp", out_d[T0 + tt * 128:T0 + (tt + 1) * 128, :], ot[:, os_, :], reads=[ok], writes=["out"])
    T.wait_all("sp")
    return nc


def _consts():
    ident = np.eye(128, dtype=np.float32)
    negi4 = np.tile(np.eye(128, dtype=np.float32) * np.float32(NEGBIG), (1, 4))
    sel = np.zeros((128, 2, 4, 96), np.float32)
    for hp in range(4):
        for d in range(32):
            sel[hp * 32 + d, 0, hp, d] = 1.0
            sel[hp * 32 + d, 0, hp, 64 + d] = 1.0
            sel[hp * 32 + d, 1, hp, 32 + d] = 1.0
    bones = np.zeros((128, 128), np.float32)
    bones[:64, :64] = 1.0 / 64
    bones[64:, 64:] = 1.0 / 64
    nrm = np.full((65, 64), 1.0 / 64, np.float32)
    nrm[64, :] = EPS
    t = np.arange(128)
    caus = np.where(t[None, :] <= t[:, None], 0.0, -1.0e30).astype(np.float32)
    pos = np.tile(np.arange(SEQ, dtype=np.float32)[None, :], (128, 1))
    return ident, negi4, sel, bones, nrm, caus, pos


def _pvec_static():
    pvc = np.zeros((128, NPV), np.float32)
    p = np.arange(128)
    inv = (np.float32(500000.0) ** (-(np.arange(0, 16, 2, dtype=np.float32) / np.float32(16)))).astype(np.float32)
    pvc[:, PV_INV] = inv[p % 8]
    pvc[:, PV_NEG1] = 1.0
    sgn = np.where(p % 16 < 8, -1.0, 1.0)
    m = (p % 32 < 16).astype(np.float32)
    pvc[:, PV_NSGN] = sgn
    pvc[:, PV_NEGM] = m
    pvc[:, PV_ONEM] = 1.0 - m
    pvc[:, PV_NSGNM] = sgn * m
    ma = ((p < 16) | ((p >= 32) & (p < 80))).astype(np.float32)
    pvc[:, PV_MA] = ma
    pvc[:, PV_MB] = 1.0 - ma
    pvc[:, PV_EPS] = EPS
    for k in range(24):
        pvc[:, PV_P2 + k] = 2.0 ** (-k)
    return pvc


def make_inputs(x, c, w_ada, b_ada, w_in, conv_w, attn_norm_g, conv_norm_g, w_out, ln1_g, ln1_b,
                w_gate, w_up, w_down, ln2_g, ln2_b, nb, ncores):
    f = np.float32
    x = np.asarray(x, f)
    c = np.asarray(c, f)
    ident, negi4, sel, bones, nrm, caus, pos = _consts()
    wa = np.asarray(w_ada[0], f)
    wada = np.ascontiguousarray(wa.reshape(8, 128, 6, 1024).transpose(2, 1, 0, 3)).reshape(6, 128, 8 * 1024)
    ba = np.asarray(b_ada[0], f)
    bbc = np.ascontiguousarray(np.broadcast_to(np.concatenate([ba[2048:3072], ba[5120:6144]])[None, :], (128, 2048)))
    wi = np.asarray(w_in[0], f)
    wi_pad = np.concatenate([wi, np.zeros((1024, 1), f)], axis=1)
    win = np.zeros((NCH, 128, 8, 128), f)
    for ci, name in enumerate(CH_ORDER):
        cols = np.array(CHUNKS[name])
        win[ci] = wi_pad[:, cols].reshape(8, 128, 128).transpose(1, 0, 2)
    win = win.reshape(NCH, 128, 8 * 128)
    wo = np.ascontiguousarray(np.asarray(w_out[0], f).reshape(8, 128, 1024).transpose(1, 0, 2))
    wg = np.asarray(w_gate[0], f).reshape(8, 128, NFF, 128)
    wu = np.asarray(w_up[0], f).reshape(8, 128, NFF, 128)
    wgu = np.concatenate([wg, wu], axis=3).transpose(2, 1, 0, 3)
    wgu = np.ascontiguousarray(wgu).reshape(NFF, 128, 8 * 256)
    wdn = np.ascontiguousarray(np.asarray(w_down[0], f).reshape(NFF, 128, 1024).transpose(1, 0, 2))
    pvc = _pvec_static()
    cw = np.asarray(conv_w[0], f)
    for j in range(4):
        for tap in range(3):
            pvc[:, PV_CW + j * 3 + tap] = cw[tap, j * 128:(j + 1) * 128]
    ag = np.asarray(attn_norm_g[0], f)
    cg = np.asarray(conv_norm_g[0], f)
    for kt in range(4):
        pvc[:, PV_G + kt] = ag[kt * 128:(kt + 1) * 128]
        pvc[:, PV_G + 4 + kt] = cg[kt * 128:(kt + 1) * 128]
    pvc[:, PV_LN1G:PV_LN1G + 8] = np.asarray(ln1_g[0], f).reshape(8, 128).T
    pvc[:, PV_LN1B:PV_LN1B + 8] = np.asarray(ln1_b[0], f).reshape(8, 128).T
    for idx, j in enumerate((0, 1, 3, 4)):
        pvc[:, PV_BADA + idx * 8:PV_BADA + idx * 8 + 8] = ba[j * 1024:(j + 1) * 1024].reshape(8, 128).T
    lnbc = np.stack([np.asarray(v[0], f) for v in (ln1_g, ln1_b, ln2_g, ln2_b)])
    lnbc = np.ascontiguousarray(np.broadcast_to(lnbc[None], (128, 4, 1024)))
    shared = {"w_ada": wada, "b_ada_bc": bbc, "w_in": win, "w_out": wo, "w_gu": wgu, "w_down": wdn, "pvec": pvc,
              "ln_bc": lnbc, "ident": ident, "negi4": negi4, "sel": sel, "bones": bones, "nrm": nrm,
              "causneg": caus, "posrow": pos}
    maps = []
    S = x.shape[1]
    for k in range(ncores):
        xs = np.ascontiguousarray(x[k * nb:(k + 1) * nb].reshape(nb * S, D))
        cs = np.ascontiguousarray(c[k * nb:(k + 1) * nb].reshape(nb, 8, 128).transpose(2, 1, 0))
        m = dict(shared)
        m["x"] = xs
        m["cT"] = cs
        maps.append(m)
    return maps


def kernel(x, c, w_ada, b_ada, w_in, conv_w, attn_norm_g, conv_norm_g, w_out, ln1_g, ln1_b,
           w_gate, w_up, w_down, ln2_g, ln2_b):
    ncores, nb = 8, 2
    maps = make_inputs(x, c, w_ada, b_ada, w_in, conv_w, attn_norm_g, conv_norm_g, w_out, ln1_g, ln1_b,
                       w_gate, w_up, w_down, ln2_g, ln2_b, nb, ncores)
    nc = build_program(nb, SEQ // 512)
    res = run_bass_kernel_spmd(nc, maps, core_ids=list(range(ncores)))
    outs = [np.asarray(r["out"], np.float32).reshape(nb, SEQ, D) for r in res.results]
    return np.concatenate(outs, axis=0)
```
